# Optimizing a Trainium2 kernel written in Bass

```python
import math
import jax
import jax.numpy as jnp
from jax import lax
import numpy as np

D_MODEL = 1024
BATCH = 4
SEQ = 8192
DEPTH = 2

GRID_W = 64
CTX_LEN = 256
HEAD_DIM = 64
D_A = D_MODEL // 2
D_B = D_MODEL - D_A
D_C = D_MODEL // 2
D_D = D_MODEL - D_C
A_HEADS = D_A // HEAD_DIM
B_HEADS = D_B // HEAD_DIM
C_HEADS = D_C // HEAD_DIM
DECAY_LORA = 64
AAA_LORA = 64
GATE_LORA = 128
LN_X_EPS = 1e-5 * HEAD_DIM
A_SPLITS = [D_A, 2 * D_A, 3 * D_A, 3 * D_A + 2 * DECAY_LORA, 3 * D_A + 2 * DECAY_LORA + 2 * AAA_LORA]
A_COLS = 3 * D_A + 2 * DECAY_LORA + 2 * AAA_LORA + GATE_LORA
B_COLS = 5 * D_B
AB_COLS = A_COLS + B_COLS
GLA_CHUNK = 64
NA_ROWS = 8
NA_COLS = 16
NA_QROWS = 2
ROPE_THETA = 10000.0
C_COLS = 3 * D_C
CD_COLS = C_COLS + 3 * D_D
HYENA_ORDER = 2
HYENA_EMB = 33
HYENA_WIDTH = 64
HYENA_FAST_DECAY = 0.3
HYENA_SLOW_DECAY = 1.5
HYENA_TARGET = 1e-2
N_EXPERTS = 32
TOP_K = 4
D_EXPERT = D_MODEL
SWIGLU_ALPHA = 1.702
SWIGLU_LIMIT = 7.0
MOE_BLOCK = 256
RMS_EPS = 1e-6

kernel_name = 'hybrid_rwkv7_hgrn2_natten_hyena_moe_dit'


def _rms(x):
    xf = x.astype(jnp.float32)
    return (xf * lax.rsqrt(jnp.mean(xf * xf, axis=-1, keepdims=True) + RMS_EPS)).astype(x.dtype)


def _modulate(x, shift, scale):
    return _rms(x) * (1 + scale) + shift


def _conv3(u, w):
    up = jnp.pad(u, ((0, 0), (1, 1), (0, 0)))
    return up[:, :-2] * w[0] + up[:, 1:-1] * w[1] + up[:, 2:] * w[2]


def _rwkv7_streams(u, shift_w, w0, w2, a0, a2, g2, k_k, k_a):
    bsz, L, _ = u.shape
    u = _conv3(u, shift_w).astype(jnp.float32)
    r, k, v, wd, ad, gd = jnp.split(u, A_SPLITS, axis=-1)
    heads = lambda t: t.reshape(t.shape[:-1] + (A_HEADS, HEAD_DIM))
    lora_w = jnp.tanh(wd.reshape(bsz, L, 2, DECAY_LORA))
    w_log = -jax.nn.softplus(-(w0 + jnp.einsum('bldr,drc->bldc', lora_w, w2))) - 0.5
    decay = jnp.exp(-jnp.exp(w_log))
    a = jax.nn.sigmoid(a0 + jnp.einsum('bldr,drc->bldc', ad.reshape(bsz, L, 2, AAA_LORA), a2))
    kk = heads(k * k_k)
    kk = kk / jnp.maximum(jnp.linalg.norm(kk, axis=-1, keepdims=True), 1e-12)
    k_dir = k[:, :, None, :] * (1.0 + (a - 1.0) * k_a)
    g = jax.nn.sigmoid(gd) @ g2
    return heads(r), heads(v), kk, heads(decay), heads(a), heads(k_dir), g


def _rwkv7_scan(s0, r, w, k, v, kk, a, reverse):
    def step(s, inp):
        r_t, w_t, k_t, v_t, kk_t, a_t = inp
        sa = jnp.einsum('bhvk,bhk->bhv', s, kk_t)
        s = (s * w_t[:, :, None, :] - sa[..., None] * (kk_t * a_t)[:, :, None, :]
             + v_t[..., None] * k_t[:, :, None, :])
        return s, jnp.einsum('bhvk,bhk->bhv', s, r_t)
    xs = tuple(jnp.swapaxes(t, 0, 1) for t in (r, w, k, v, kk, a))
    s_fin, y = lax.scan(step, s0, xs, reverse=reverse)
    return jnp.swapaxes(y, 0, 1), s_fin


def _rwkv7_dir_args(streams, d):
    r, v, kk, decay, a, k_dir, _ = streams
    return r, decay[:, :, d], k_dir[:, :, d], v, kk, a[:, :, d]


def _rwkv7_readout(y, streams, r_k, ln_w, ln_b):
    r, v, _, _, _, k_dir, g = streams
    bsz, L = y.shape[:2]
    mu = jnp.mean(y, axis=-1, keepdims=True)
    var = jnp.mean(jnp.square(y - mu), axis=-1, keepdims=True)
    y = ((y - mu) * lax.rsqrt(var + LN_X_EPS)).reshape(bsz, L, D_A) * ln_w + ln_b
    bonus = jnp.sum(jnp.sum(r[:, :, None] * k_dir * r_k, axis=-1, keepdims=True) * v[:, :, None], axis=2)
    return (y + bonus.reshape(bsz, L, D_A)) * g


def _rwkv7_mix(u, uc, need_ctx, shift_w, w0, w2, a0, a2, g2, k_k, k_a, r_k, ln_w, ln_b):
    lat = _rwkv7_streams(u, shift_w, w0, w2, a0, a2, g2, k_k, k_a)
    ctx = _rwkv7_streams(uc, shift_w, w0, w2, a0, a2, g2, k_k, k_a)
    s0 = jnp.zeros((u.shape[0], A_HEADS, HEAD_DIM, HEAD_DIM), jnp.float32)
    y_dirs, yc_dirs = [], []
    for d in range(2):
        yc_d, s_ctx = _rwkv7_scan(s0, *_rwkv7_dir_args(ctx, d), reverse=(d == 1))
        y_d, _ = _rwkv7_scan(s_ctx, *_rwkv7_dir_args(lat, d), reverse=(d == 1))
        y_dirs.append(y_d)
        yc_dirs.append(yc_d)
    y = _rwkv7_readout(y_dirs[0] + y_dirs[1], lat, r_k, ln_w, ln_b)
    yc = _rwkv7_readout(yc_dirs[0] + yc_dirs[1], ctx, r_k, ln_w, ln_b) if need_ctx else None
    return y, yc


def _gla_chunked(q, k, v, logf, s0):
    bsz, L, H, _ = q.shape
    n = L // GLA_CHUNK
    def chunks(t):
        return t.reshape(bsz, n, GLA_CHUNK, H, t.shape[-1]).transpose(1, 0, 3, 2, 4)
    qc, kc, vc, gc = chunks(q), chunks(k), chunks(v), chunks(logf)
    b = jnp.cumsum(gc, axis=3)
    b_mid = b[:, :, :, GLA_CHUNK // 2 - 1:GLA_CHUNK // 2]
    att = jnp.einsum('nbhtd,nbhsd->nbhts', qc * jnp.exp(b - b_mid), kc * jnp.exp(b_mid - b))
    within = jnp.tril(jnp.ones((GLA_CHUNK, GLA_CHUNK), dtype=bool))
    o_intra = jnp.einsum('nbhts,nbhse->nbhte', jnp.where(within, att, 0.0), vc)
    q_in = qc * jnp.exp(b)
    k_out = kc * jnp.exp(b[:, :, :, -1:] - b)
    dec = jnp.exp(b[:, :, :, -1])
    def step(s, inp):
        q_t, k_t, v_t, d_t = inp
        o = jnp.einsum('bhtd,bhde->bhte', q_t, s)
        s = s * d_t[..., None] + jnp.einsum('bhsd,bhse->bhde', k_t, v_t)
        return s, o
    s_fin, o_inter = lax.scan(step, s0, (q_in, k_out, vc, dec))
    o = (o_intra + o_inter).transpose(1, 0, 3, 2, 4).reshape(bsz, L, H, v.shape[-1])
    return o, s_fin


def _gla_dir(q, k, v, logf, s0, reverse):
    if not reverse:
        return _gla_chunked(q, k, v, logf, s0)
    flip = lambda t: jnp.flip(t, axis=1)
    o, s_fin = _gla_chunked(flip(q), flip(k), flip(v), flip(logf), s0)
    return flip(o), s_fin


def _hgrn2_streams(u, lb):
    bsz, L, _ = u.shape
    q, f_fwd, f_bwd, i, og = jnp.split(u.astype(jnp.float32), 5, axis=-1)
    heads = lambda t: t.reshape(bsz, L, B_HEADS, HEAD_DIM)
    dirs = []
    for f in (f_fwd, f_bwd):
        fg = lb + (1.0 - lb) * jax.nn.sigmoid(f)
        dirs.append((heads(1.0 - fg), heads(jnp.log(fg))))
    return heads(jax.nn.silu(q)), heads(i), og, dirs


def _hgrn2_readout(o, og, norm_w):
    return (_rms(o) * norm_w).reshape(o.shape[:2] + (D_B,)) * jax.nn.silu(og)


def _hgrn2_mix(u, uc, need_ctx, lb, norm_w):
    q, i, og, dirs = _hgrn2_streams(u, lb)
    qc, ic, ogc, dirs_c = _hgrn2_streams(uc, lb)
    s0 = jnp.zeros((u.shape[0], B_HEADS, HEAD_DIM, HEAD_DIM), jnp.float32)
    o_dirs, oc_dirs = [], []
    for d in range(2):
        kc_d, lfc_d = dirs_c[d]
        k_d, lf_d = dirs[d]
        oc_d, s_ctx = _gla_dir(qc, kc_d, ic, lfc_d, s0, reverse=(d == 1))
        o_d, _ = _gla_dir(q, k_d, i, lf_d, s_ctx, reverse=(d == 1))
        o_dirs.append(o_d)
        oc_dirs.append(oc_d)
    y = _hgrn2_readout(o_dirs[0] + o_dirs[1], og, norm_w)
    yc = _hgrn2_readout(oc_dirs[0] + oc_dirs[1], ogc, norm_w) if need_ctx else None
    return y, yc


def _even_mixer(h, hc, need_ctx, w_in, w_out, shift_w, w0, w2, a0, a2, g2, k_k, k_a, r_k, ln_w, ln_b,
                lb, norm_w):
    u, uc = h @ w_in, hc @ w_in
    ya, yac = _rwkv7_mix(u[..., :A_COLS], uc[..., :A_COLS], need_ctx, shift_w, w0, w2, a0, a2, g2,
                         k_k, k_a, r_k, ln_w, ln_b)
    yb, ybc = _hgrn2_mix(u[..., A_COLS:], uc[..., A_COLS:], need_ctx, lb, norm_w)
    y = jnp.concatenate([ya, yb], axis=-1).astype(h.dtype) @ w_out
    if not need_ctx:
        return y, None
    yc = jnp.concatenate([yac, ybc], axis=-1).astype(h.dtype) @ w_out
    return y, yc


def _axial_rope(x):
    bsz, L, H, dh = x.shape
    t = jnp.arange(L)
    pos = jnp.stack([t // GRID_W, t % GRID_W], axis=-1).astype(jnp.float32)
    nf = dh // 4
    inv = ROPE_THETA ** (-jnp.arange(nf, dtype=jnp.float32) / nf)
    ang = pos[:, None, :, None] * inv
    cos, sin = jnp.cos(ang), jnp.sin(ang)
    xr = x.astype(jnp.float32).reshape(bsz, L, H, 2, 2, nf)
    x1, x2 = xr[..., 0, :], xr[..., 1, :]
    out = jnp.stack([x1 * cos - x2 * sin, x1 * sin + x2 * cos], axis=-2)
    return out.reshape(bsz, L, H, dh).astype(x.dtype)


def _na_latent(q, k, v, kc, vc, rpb):
    bsz, S, H, dh = q.shape
    rows = S // GRID_W
    wr = min(NA_ROWS, rows)
    r = np.arange(rows)
    row_idx = np.clip(r - wr // 2, 0, rows - wr)[:, None] + np.arange(wr)
    row_off = row_idx - r[:, None] + NA_ROWS - 1
    cc = np.arange(GRID_W)
    col_idx = np.clip(cc - NA_COLS // 2, 0, GRID_W - NA_COLS)[:, None] + np.arange(NA_COLS)
    col_off = col_idx - cc[:, None] + NA_COLS - 1
    n_blk = rows // NA_QROWS
    qg = q.reshape(bsz, n_blk, NA_QROWS, GRID_W, H, dh).transpose(1, 0, 2, 3, 4, 5)
    kg = k.reshape(bsz, rows, GRID_W, H, dh)
    vg = v.reshape(bsz, rows, GRID_W, H, dh)
    ridx = jnp.asarray(row_idx.reshape(n_blk, NA_QROWS, wr))
    roff = jnp.asarray(row_off.reshape(n_blk, NA_QROWS, wr))
    n_win = wr * NA_COLS

    def block(args):
        qb, ri, ro = args
        kw = kg[:, ri][:, :, :, col_idx]
        vw = vg[:, ri][:, :, :, col_idx]
        s_win = jnp.einsum('bqwhd,bqrwchd->bhqwrc', qb, kw)
        bias = rpb[:, ro[:, None, :, None], col_off[None, :, None, :]]
        s_win = (s_win + bias[None]).reshape(bsz, H, NA_QROWS, GRID_W, n_win)
        s_ctx = jnp.einsum('bqwhd,bkhd->bhqwk', qb, kc)
        p = jax.nn.softmax(jnp.concatenate([s_win, s_ctx], axis=-1).astype(jnp.float32), axis=-1)
        p_win = p[..., :n_win].reshape(bsz, H, NA_QROWS, GRID_W, wr, NA_COLS).astype(v.dtype)
        p_ctx = p[..., n_win:].astype(v.dtype)
        return (jnp.einsum('bhqwrc,bqrwchd->bqwhd', p_win, vw)
                + jnp.einsum('bhqwk,bkhd->bqwhd', p_ctx, vc))

    o = lax.map(block, (qg, ridx, roff))
    return o.transpose(1, 0, 2, 3, 4, 5).reshape(bsz, S, H * dh)


def _ctx_attention(q, k, v):
    s = jnp.einsum('bqhd,bkhd->bhqk', q, k).astype(jnp.float32)
    p = jax.nn.softmax(s, axis=-1).astype(v.dtype)
    o = jnp.einsum('bhqk,bkhd->bqhd', p, v)
    return o.reshape(o.shape[:2] + (-1,))


def _hyena_filters(L, w1, b1, fr1, w2, b2, fr2, w3):
    t = jnp.linspace(0.0, 1.0, L, dtype=jnp.float32)[:, None]
    bands = (HYENA_EMB - 1) // 2
    f = jnp.linspace(1e-4, bands - 1, bands, dtype=jnp.float32)
    ang = (2.0 * math.pi / L) * jnp.arange(L, dtype=jnp.float32)[:, None] * f
    z = jnp.concatenate([t, jnp.cos(ang), -jnp.sin(ang)], axis=-1)
    hid = jnp.sin(fr1 * (z @ w1 + b1))
    hid = jnp.sin(fr2 * (hid @ w2 + b2))
    h = (hid @ w3).reshape(L, HYENA_ORDER, 2, D_D)
    deltas = jnp.abs(jnp.linspace(math.log(HYENA_TARGET) / HYENA_SLOW_DECAY,
                                  math.log(HYENA_TARGET) / HYENA_FAST_DECAY, D_D, dtype=jnp.float32))
    return h * jnp.exp(-t * deltas)[:, None, None, :]


def _bidir_longconv(z, h_fwd, h_bwd, bias):
    L = z.shape[1]
    k2 = jnp.concatenate([h_fwd.at[0].add(h_bwd[0]), jnp.zeros_like(h_fwd[:1]), h_bwd[:0:-1]], axis=0)
    zf = jnp.fft.rfft(z.astype(jnp.float32), n=2 * L, axis=1)
    kf = jnp.fft.rfft(k2.astype(jnp.float32), n=2 * L, axis=0)
    y = jnp.fft.irfft(zf * kf, n=2 * L, axis=1)[:, :L]
    return (y + z * bias).astype(z.dtype)


def _hyena(u, short_w, w1, b1, fr1, w2, b2, fr2, w3, bias):
    L = u.shape[1]
    u = _conv3(u, short_w)
    v, x1, x2 = jnp.split(u, 3, axis=-1)
    h = _hyena_filters(L, w1, b1, fr1, w2, b2, fr2, w3)
    z = x1 * _bidir_longconv(v, h[:, 0, 0], h[:, 0, 1], bias[0])
    return x2 * _bidir_longconv(z, h[:, 1, 0], h[:, 1, 1], bias[1])


def _odd_mixer(h, hc, need_ctx, w_in, w_out, q_norm, k_norm, rpb, short_w, w1, b1, fr1, w2, b2, fr2, w3,
               bias):
    u, uc = h @ w_in, hc @ w_in

    def qkv(t):
        bsz, L, _ = t.shape
        q, k, v = jnp.split(t[..., :C_COLS], 3, axis=-1)
        heads = lambda a: a.reshape(bsz, L, C_HEADS, HEAD_DIM)
        return _rms(heads(q)) * q_norm, _rms(heads(k)) * k_norm, heads(v)

    q, k, v = qkv(u)
    qc, kc, vc = qkv(uc)
    scale = HEAD_DIM ** -0.5
    y_na = _na_latent(_axial_rope(q) * scale, _axial_rope(k), v, kc, vc, rpb)
    y_hy = _hyena(u[..., C_COLS:], short_w, w1, b1, fr1, w2, b2, fr2, w3, bias)
    y = jnp.concatenate([y_na, y_hy.astype(y_na.dtype)], axis=-1) @ w_out
    if not need_ctx:
        return y, None
    yc_na = _ctx_attention(qc * scale, kc, vc)
    yc_hy = _hyena(uc[..., C_COLS:], short_w, w1, b1, fr1, w2, b2, fr2, w3, bias)
    yc = jnp.concatenate([yc_na, yc_hy.astype(yc_na.dtype)], axis=-1) @ w_out
    return y, yc


def _clamped_swiglu(y):
    glu, lin = y[..., ::2], y[..., 1::2]
    glu = jnp.minimum(glu, SWIGLU_LIMIT)
    lin = jnp.clip(lin, -SWIGLU_LIMIT, SWIGLU_LIMIT)
    return glu * jax.nn.sigmoid(SWIGLU_ALPHA * glu) * (lin + 1.0)


def _moe(h, router_w, router_b, w1, b1, w2, b2):
    T, D = h.shape
    logits = (h @ router_w + router_b).astype(jnp.float32)
    top_logit, top_e = lax.top_k(logits, TOP_K)
    gate = jax.nn.softmax(top_logit, axis=-1)
    n_assign = T * TOP_K
    flat_e = top_e.reshape(-1)
    order = jnp.argsort(flat_e)
    e_sorted = flat_e[order]
    counts = jnp.bincount(flat_e, length=N_EXPERTS)
    padded = (counts + MOE_BLOCK - 1) // MOE_BLOCK * MOE_BLOCK
    start = jnp.cumsum(counts) - counts
    p_end = jnp.cumsum(padded)
    p_start = p_end - padded
    dest = p_start[e_sorted] + jnp.arange(n_assign) - start[e_sorted]
    n_blocks = -(-n_assign // MOE_BLOCK) + N_EXPERTS
    n_slots = n_blocks * MOE_BLOCK
    slot_tok = jnp.full((n_slots,), T, jnp.int32).at[dest].set((order // TOP_K).astype(jnp.int32))
    slot_gate = jnp.zeros((n_slots,), jnp.float32).at[dest].set(gate.reshape(-1)[order])
    blk_e = jnp.minimum(jnp.searchsorted(p_end, jnp.arange(n_blocks) * MOE_BLOCK, side='right'),
                        N_EXPERTS - 1)
    x_slots = jnp.concatenate([h, jnp.zeros((1, D), h.dtype)], axis=0)[slot_tok]
    x_slots = x_slots.reshape(n_blocks, MOE_BLOCK, D)

    def expert_block(args):
        xb, e = args
        y = _clamped_swiglu(xb @ w1[e] + b1[e])
        return y @ w2[e] + b2[e]

    y = lax.map(expert_block, (x_slots, blk_e)).reshape(n_slots, D)
    out = jnp.zeros((T + 1, D), jnp.float32).at[slot_tok].add(y.astype(jnp.float32) * slot_gate[:, None])
    return out[:T].astype(h.dtype)


def setup_inputs(seed: int = 0) -> dict:
    key = jax.random.key(seed)
    ks = iter(jax.random.split(key, 48))
    def nrm(shape, scale=1.0):
        return scale * jax.random.normal(next(ks), shape, jnp.float32)
    ne, no = (DEPTH + 1) // 2, DEPTH // 2
    taps = jnp.array([0.25, 0.5, 0.25], jnp.float32)[:, None]
    return {
        'x': nrm((BATCH, SEQ, D_MODEL)),
        'c': nrm((BATCH, D_MODEL)),
        'ctx': nrm((BATCH, CTX_LEN, D_MODEL)),
        'c_ctx': nrm((D_MODEL,)),
        'mod_w': nrm((DEPTH, D_MODEL, 6 * D_MODEL), D_MODEL ** -0.5),
        'mod_b': nrm((DEPTH, 6 * D_MODEL), 0.02),
        'router_w': nrm((DEPTH, D_MODEL, N_EXPERTS), D_MODEL ** -0.5),
        'router_b': nrm((DEPTH, N_EXPERTS), 0.01),
        'moe_w1': nrm((DEPTH, N_EXPERTS, D_MODEL, 2 * D_EXPERT), D_MODEL ** -0.5),
        'moe_b1': nrm((DEPTH, N_EXPERTS, 2 * D_EXPERT), 0.02),
        'moe_w2': nrm((DEPTH, N_EXPERTS, D_EXPERT, D_MODEL), D_EXPERT ** -0.5),
        'moe_b2': nrm((DEPTH, N_EXPERTS, D_MODEL), 0.02),
        'ab_w_in': nrm((ne, D_MODEL, AB_COLS), D_MODEL ** -0.5),
        'ab_w_out': nrm((ne, D_A + D_B, D_MODEL), (D_A + D_B) ** -0.5),
        'rwkv_shift': taps + nrm((ne, 3, A_COLS), 0.05),
        'rwkv_w0': jnp.linspace(-6.0, -1.0, D_A, dtype=jnp.float32) + nrm((ne, 2, D_A), 0.1),
        'rwkv_w2': nrm((ne, 2, DECAY_LORA, D_A), 0.1),
        'rwkv_a0': nrm((ne, 2, D_A), 0.1),
        'rwkv_a2': nrm((ne, 2, AAA_LORA, D_A), 0.1),
        'rwkv_g2': nrm((ne, GATE_LORA, D_A), GATE_LORA ** -0.5),
        'rwkv_k_k': 0.85 + nrm((ne, D_A), 0.05),
        'rwkv_k_a': 1.0 + nrm((ne, D_A), 0.05),
        'rwkv_r_k': nrm((ne, A_HEADS, HEAD_DIM), 0.1),
        'rwkv_ln_w': 1.0 + nrm((ne, D_A), 0.05),
        'rwkv_ln_b': nrm((ne, D_A), 0.02),
        'hgrn_lb_logits': nrm((ne + 1, D_B), 0.1),
        'hgrn_norm_w': 1.0 + nrm((ne, HEAD_DIM), 0.05),
        'cd_w_in': nrm((no, D_MODEL, CD_COLS), D_MODEL ** -0.5),
        'cd_w_out': nrm((no, D_C + D_D, D_MODEL), (D_C + D_D) ** -0.5),
        'na_q_norm': 1.0 + nrm((no, HEAD_DIM), 0.05),
        'na_k_norm': 1.0 + nrm((no, HEAD_DIM), 0.05),
        'na_rpb': nrm((no, C_HEADS, 2 * NA_ROWS - 1, 2 * NA_COLS - 1), 0.1),
        'hy_short': taps + nrm((no, 3, 3 * D_D), 0.05),
        'hy_w1': nrm((no, HYENA_EMB, HYENA_WIDTH), HYENA_EMB ** -0.5),
        'hy_b1': nrm((no, HYENA_WIDTH), 0.1),
        'hy_freq1': 1.0 + nrm((no, HYENA_WIDTH), 0.1),
        'hy_w2': nrm((no, HYENA_WIDTH, HYENA_WIDTH), HYENA_WIDTH ** -0.5),
        'hy_b2': nrm((no, HYENA_WIDTH), 0.1),
        'hy_freq2': 1.0 + nrm((no, HYENA_WIDTH), 0.1),
        'hy_w3': nrm((no, HYENA_WIDTH, HYENA_ORDER * 2 * D_D), 0.004),
        'hy_bias': nrm((no, HYENA_ORDER, D_D)),
    }


def reference(x, c, ctx, c_ctx, mod_w, mod_b, router_w, router_b, moe_w1, moe_b1, moe_w2, moe_b2,
              ab_w_in, ab_w_out, rwkv_shift, rwkv_w0, rwkv_w2, rwkv_a0, rwkv_a2, rwkv_g2, rwkv_k_k,
              rwkv_k_a, rwkv_r_k, rwkv_ln_w, rwkv_ln_b, hgrn_lb_logits, hgrn_norm_w, cd_w_in, cd_w_out,
              na_q_norm, na_k_norm, na_rpb, hy_short, hy_w1, hy_b1, hy_freq1, hy_w2, hy_b2, hy_freq2,
              hy_w3, hy_bias):
    xc = ctx
    lb_all = jnp.cumsum(jax.nn.softmax(hgrn_lb_logits.astype(jnp.float32), axis=0), axis=0)
    for l in range(DEPTH):
        need_ctx = l < DEPTH - 1
        j = l // 2
        mod = jax.nn.silu(c) @ mod_w[l] + mod_b[l]
        mod_c = jax.nn.silu(c_ctx) @ mod_w[l] + mod_b[l]
        sh1, sc1, g1, sh2, sc2, g2 = jnp.split(mod[:, None, :], 6, axis=-1)
        sh1c, sc1c, g1c, sh2c, sc2c, g2c = jnp.split(mod_c, 6)
        h = _modulate(x, sh1, sc1)
        hc = _modulate(xc, sh1c, sc1c)
        if l % 2 == 0:
            y, yc = _even_mixer(h, hc, need_ctx, ab_w_in[j], ab_w_out[j], rwkv_shift[j], rwkv_w0[j],
                                rwkv_w2[j], rwkv_a0[j], rwkv_a2[j], rwkv_g2[j], rwkv_k_k[j], rwkv_k_a[j],
                                rwkv_r_k[j], rwkv_ln_w[j], rwkv_ln_b[j], lb_all[j], hgrn_norm_w[j])
        else:
            y, yc = _odd_mixer(h, hc, need_ctx, cd_w_in[j], cd_w_out[j], na_q_norm[j], na_k_norm[j],
                               na_rpb[j], hy_short[j], hy_w1[j], hy_b1[j], hy_freq1[j], hy_w2[j], hy_b2[j],
                               hy_freq2[j], hy_w3[j], hy_bias[j])
        x = x + g1 * y.astype(x.dtype)
        h = _modulate(x, sh2, sc2)
        n_lat = h.shape[0] * h.shape[1]
        if need_ctx:
            xc = xc + g1c * yc.astype(xc.dtype)
            hc = _modulate(xc, sh2c, sc2c)
            tokens = jnp.concatenate([h.reshape(n_lat, -1), hc.reshape(-1, hc.shape[-1])], axis=0)
            out = _moe(tokens, router_w[l], router_b[l], moe_w1[l], moe_b1[l], moe_w2[l], moe_b2[l])
            x = x + g2 * out[:n_lat].reshape(x.shape)
            xc = xc + g2c * out[n_lat:].reshape(xc.shape)
        else:
            out = _moe(h.reshape(n_lat, -1), router_w[l], router_b[l], moe_w1[l], moe_b1[l], moe_w2[l],
                       moe_b2[l])
            x = x + g2 * out.reshape(x.shape)
    return x
```

```python
import math
import os
from concourse.bass_utils import run_bass_kernel_spmd
import numpy as np
from contextlib import ExitStack
import concourse.bass as bass
import concourse.mybir as mybir

F32 = mybir.dt.float32
BF16 = mybir.dt.bfloat16
I32 = mybir.dt.int32
AF = mybir.ActivationFunctionType
ALU = mybir.AluOpType
AX = mybir.AxisListType

SEM_ROLL = 30000
N_DMA_SEMS = 12


class Buf:
    __slots__ = ("name", "w", "r", "excl", "rg")

    def __init__(self, name="", excl=False):
        self.name = name
        self.excl = excl
        self.rg = None
        self.w = None
        self.r = []


class V:
    __slots__ = ("ap", "buf")

    def __init__(self, ap, buf):
        self.ap = ap
        self.buf = buf

    def __getitem__(self, idx):
        return V(self.ap[idx], self.buf)

    def re(self, pat, **kw):
        return V(self.ap.rearrange(pat, **kw), self.buf)

    def with_ap(self, ap):
        return V(ap, self.buf)


class Prog:
    ENG = ("sp", "act", "dve", "pool", "pe")

    def __init__(self, nc):
        self.nc = nc
        self.es = ExitStack()
        self.streams = {e: [] for e in self.ENG}
        self.sem = {}
        self.cnt = {}
        for e in self.ENG:
            self.sem[e] = self._newsem("s_" + e)
            self.cnt[e] = 0
        self.seen = {e: {} for e in self.ENG}
        self.dsems = [self._newsem("d%d" % i) for i in range(N_DMA_SEMS)]
        self.dcnt = [0] * N_DMA_SEMS
        self.dnext = 0
        self.nbuf = 0
        self.out_tokens = []
        self.pstack = None

    def _newsem(self, name):
        self._semid = getattr(self, "_semid", 0) + 1
        return self.es.enter_context(self.nc.semaphore("%s_%d" % (name, self._semid)))

    def sbuf(self, shape, dt=F32, name=None):
        self.nbuf += 1
        name = "%s_%d" % (name or "t", self.nbuf)
        t = (self.pstack or self.es).enter_context(self.nc.sbuf_tensor("sb_" + name, list(shape), dt))
        return V(t[:] if False else t, Buf(name)) if False else V(_full(t), Buf(name))

    def psum(self, shape, dt=F32, name=None):
        self.nbuf += 1
        name = name or "p%d" % self.nbuf
        t = self.es.enter_context(self.nc.psum_tensor("ps_" + name, list(shape), dt))
        return V(_full(t), Buf(name, excl=True))

    def dram(self, name, shape, dt=F32, kind="Internal"):
        t = self.nc.dram_tensor(name, list(shape), dt, kind=kind)
        return V(t.ap(), Buf(name))

    def split(self, v, n):
        bufs = [Buf("%s_%d" % (getattr(v.buf, "name", "v"), i)) for i in range(n)]
        subs = [V(v.ap[:, i], bufs[i]) for i in range(n)]
        return subs, V(v.ap, bufs)

    def _deps(self, eng, reads, writes, xeng=False):
        waits = {}

        def need(tok):
            if tok is None:
                return
            teng, sem, val = tok
            if teng == eng and teng != "dma":
                pass
            key = sem.name
            if self.seen[eng].get(key, -1) >= val:
                return
            if key not in waits or waits[key][1] < val:
                waits[key] = (sem, val)

        for b in reads:
            need(b.w)
        for b in writes:
            if not (b.w is not None and b.w[0] == eng and eng != "dma" and not (b.excl and False)):
                need(b.w)
            for t in b.r:
                if t[0] == eng and eng != "dma":
                    continue
                need(t)
        for key, (sem, val) in waits.items():
            self.seen[eng][key] = val
        return list(waits.values())

    def _record(self, tok, reads, writes):
        for b in reads:
            b.r.append(tok)
        for b in writes:
            b.w = tok
            b.r = []

    def op(self, eng, fn, outs, ins, force=()):
        reads = _bufs(ins)
        writes = _bufs(outs)
        for b in list(reads):
            if b.excl:
                reads.remove(b)
                if b not in writes:
                    writes.append(b)
        waits = self._deps(eng, reads, writes, xeng=True)
        for (te, sem, val) in force:
            if self.seen[eng].get(sem.name, -1) < val:
                self.seen[eng][sem.name] = val
                waits.append((sem, val))
        if self.cnt[eng] >= SEM_ROLL:
            self.sem[eng] = self._newsem("s_" + eng)
            self.cnt[eng] = 0
        self.cnt[eng] += 1
        tok = (eng, self.sem[eng], self.cnt[eng])
        self.streams[eng].append((fn, waits, self.sem[eng], 1))
        self._record(tok, reads, writes)
        return tok

    def dma(self, out, in_, q="sp", **kw):
        if q == "pool":
            q = "sp"
        reads = _bufs([in_])
        writes = _bufs([out])
        k = self.dnext
        self.dnext = (self.dnext + 1) % N_DMA_SEMS
        sem = self.dsems[k]
        waits = self._deps(q, reads, writes)
        if self.dcnt[k] > 0 and self.seen[q].get(sem.name, -1) < self.dcnt[k]:
            waits.append((sem, self.dcnt[k]))
            self.seen[q][sem.name] = self.dcnt[k]
        self.dcnt[k] += 16
        tok = ("dma", sem, self.dcnt[k])
        o_ap, i_ap = out.ap, in_.ap
        self.streams[q].append((lambda e: e.dma_start(out=o_ap, in_=i_ap, **kw), waits, sem, 16))
        self._record(tok, reads, writes)
        return tok

    def cc(self, kind, out, in_, groups, op=None, inc=1):
        reads = _bufs([in_])
        writes = _bufs([out])
        waits = self._deps("pool", reads, writes)
        sem = self._newsem("cc")
        tok = ("dma", sem, inc)
        o_ap, i_ap = out.ap.opt(), in_.ap.opt()
        aop = op if op is not None else ALU.bypass
        self.streams["pool"].append((lambda e: e.collective_compute(kind, aop, replica_groups=groups, ins=[i_ap], outs=[o_ap]), waits, sem, inc))
        self._record(tok, reads, writes)
        if not hasattr(self, "cc_toks"):
            self.cc_toks = []
        self.cc_toks.append((sem, inc))
        return tok

    def mm(self, out, lhsT, rhs, start=True, stop=True, **kw):
        o, l, r = out.ap, lhsT.ap, rhs.ap
        rg = l.base_partition()
        force = []
        for b in _bufs([out]):
            if b.rg is not None and b.rg != rg and b.w is not None and b.w[0] == "pe":
                force.append(b.w)
            b.rg = rg
        return self.op("pe", lambda e: e.matmul(o, l, r, start=start, stop=stop, **kw), [out], [lhsT, rhs], force=force)

    def tr(self, out, in_, ident):
        o, i, d = out.ap, in_.ap, ident.ap
        return self.op("pe", lambda e: e.transpose(o, i, d), [out], [in_, ident])

    def act(self, out, in_, func, bias=0.0, scale=1.0, eng="act", accum_out=None):
        o, i = out.ap, in_.ap
        ins = [in_]
        outs = [out]
        b = bias
        if isinstance(bias, V):
            ins.append(bias); b = bias.ap
        s = scale
        if isinstance(scale, V):
            ins.append(scale); s = scale.ap
        kw = {}
        if accum_out is not None:
            outs.append(accum_out); kw["accum_out"] = accum_out.ap
        return self.op(eng, lambda e: e.activation(o, i, func, bias=b, scale=s, **kw), outs, ins)

    def tt(self, out, in0, in1, op, eng="dve"):
        o, a, b = out.ap, in0.ap, in1.ap
        return self.op(eng, lambda e: e.tensor_tensor(o, a, b, op), [out], [in0, in1])

    def ts(self, out, in0, s1, op0, s2=None, op1=None, eng="dve", accum_out=None):
        o, a = out.ap, in0.ap
        ins = [in0]
        outs = [out]
        a1 = s1
        if isinstance(s1, V):
            ins.append(s1); a1 = s1.ap
        a2 = s2
        if isinstance(s2, V):
            ins.append(s2); a2 = s2.ap
        kw = {}
        if op1 is not None:
            kw["op1"] = op1
        if accum_out is not None:
            outs.append(accum_out); kw["accum_out"] = accum_out.ap
        return self.op(eng, lambda e: e.tensor_scalar(o, a, a1, a2, op0, **kw), outs, ins)

    def stt(self, out, in0, scalar, in1, op0, op1, eng="dve"):
        o, a, b = out.ap, in0.ap, in1.ap
        ins = [in0, in1]
        s = scalar
        if isinstance(scalar, V):
            ins.append(scalar); s = scalar.ap
        return self.op(eng, lambda e: e.scalar_tensor_tensor(o, a, s, b, op0, op1), [out], ins)

    def copy(self, out, in_, eng="dve"):
        o, i = out.ap, in_.ap
        if eng == "act":
            return self.op(eng, lambda e: e.copy(o, i), [out], [in_])
        return self.op(eng, lambda e: e.tensor_copy(o, i), [out], [in_])

    def memset(self, out, val, eng="dve"):
        o = out.ap
        return self.op(eng, lambda e: e.memset(o, val), [out], [])

    def reduce(self, out, in_, op, axis=AX.X, eng="dve"):
        o, i = out.ap, in_.ap
        return self.op(eng, lambda e: e.tensor_reduce(o, i, axis, op), [out], [in_])

    def recip(self, out, in_):
        o, i = out.ap, in_.ap
        return self.op("dve", lambda e: e.reciprocal(o, i), [out], [in_])

    def scan(self, out, d0, d1, initial, op0, op1):
        o, a, b = out.ap, d0.ap, d1.ap
        ins = [d0, d1]
        init = initial
        if isinstance(initial, V):
            ins.append(initial); init = initial.ap
        return self.op("dve", lambda e: e.tensor_tensor_scan(o, a, b, init, op0, op1), [out], ins)

    def barrier(self):
        toks = [(self.sem[e], self.cnt[e]) for e in self.ENG if self.cnt[e] > 0]
        toks += [(self.dsems[k], self.dcnt[k]) for k in range(N_DMA_SEMS) if self.dcnt[k] > 0]
        toks += list(getattr(self, "cc_toks", []))
        for e in self.ENG:
            waits = []
            for (sem, val) in toks:
                if sem is self.sem[e]:
                    continue
                if self.seen[e].get(sem.name, -1) >= val:
                    continue
                self.seen[e][sem.name] = val
                waits.append((sem, val))
            if waits:
                self.streams[e].append((None, waits, None, 0))

    def flush(self):
        nc = self.nc
        streams = self.streams

        def replay(ename, e):
            for fn, waits, sem, inc in streams[ename]:
                for (s, v) in waits:
                    e.wait_ge(s, v)
                if fn is not None:
                    fn(e).then_inc(sem, inc)

        if any(len(v) for v in streams.values()):
            with nc.Block() as block:
                @block.sync
                def _(e):
                    replay("sp", e)

                @block.scalar
                def _(e):
                    replay("act", e)

                @block.vector
                def _(e):
                    replay("dve", e)

                @block.gpsimd
                def _(e):
                    replay("pool", e)

                @block.tensor
                def _(e):
                    replay("pe", e)
        self.streams = {e: [] for e in self.ENG}

    class _Phase:
        def __init__(self, prog):
            self.p = prog

        def __enter__(self):
            self.p.pstack = ExitStack()
            return self.p

        def __exit__(self, *a):
            if a[0] is None:
                self.p.barrier()
                self.p.flush()
            self.p.pstack.close()
            self.p.pstack = None
            return False

    def phase(self):
        return Prog._Phase(self)

    def finish(self, final_bufs=None):
        self.barrier()
        self.flush()
        self.es.close()


def _full(t):
    nd = len(t.shape)
    return t[tuple(slice(None) for _ in range(nd))]


def _bufs(lst):
    out = []
    for x in lst:
        if x is None:
            continue
        b = x.buf if isinstance(x, V) else x
        bl = b if isinstance(b, (list, tuple)) else [b]
        for bb in bl:
            if bb not in out:
                out.append(bb)
    return out


D = 1024
NE = 32
RMS_EPS = 1e-6


def build_ffn(n_lat, n_ctx, n_exp=NE):
    nc = bass.Bass("TRN2", target_bir_lowering=False)
    p = Prog(nc)
    xT = p.dram("xT", [D, n_lat], kind="ExternalInput")
    ymT = p.dram("ymT", [D, n_lat], kind="ExternalInput")
    xoT = p.dram("xoT", [D, n_lat], kind="ExternalOutput")
    if n_ctx:
        xcT = p.dram("xcT", [D, n_ctx], kind="ExternalInput")
        ymcT = p.dram("ymcT", [D, n_ctx], kind="ExternalInput")
        xocT = p.dram("xocT", [D, n_ctx], kind="ExternalOutput")
    cvec = p.dram("cvec", [128, 8, 2], kind="ExternalInput")
    modw = p.dram("modw", [D, 4 * D], kind="ExternalInput")
    modb = p.dram("modb", [128, 48], kind="ExternalInput")
    wout = p.dram("wout", [D, D], kind="ExternalInput")
    rw = p.dram("rw", [D, NE], kind="ExternalInput")
    rb = p.dram("rb", [1, NE], kind="ExternalInput")
    w1 = p.dram("w1", [NE, D, 2 * D], kind="ExternalInput")
    b1 = p.dram("b1", [128, NE * 16], kind="ExternalInput")
    w2 = p.dram("w2", [NE, D, D], kind="ExternalInput")
    b2 = p.dram("b2", [NE, D], kind="ExternalInput")
    identd = p.dram("ident", [128, 128], kind="ExternalInput")

    ident = p.sbuf([128, 128], F32, "ident")
    onesb = p.sbuf([128, 128], BF16, "onesb")
    woutb = p.sbuf([128, 8, D], BF16, "woutb")
    rwt = p.sbuf([128, 8, NE], F32, "rwt")
    rbt = p.sbuf([128, NE], F32, "rbt")
    b1t = p.sbuf([128, NE * 16], F32, "b1t")
    b2t = p.sbuf([NE, D], F32, "b2t")
    cs = p.sbuf([128, 8, 2], F32, "cs")
    modT = p.sbuf([128, 32, 2], F32, "modT")
    modbt = p.sbuf([128, 48], F32, "modbt")
    epst = p.sbuf([128, 1], F32, "epst")
    p.dma(ident, identd)
    p.memset(onesb, 1.0)
    p.memset(epst, RMS_EPS)
    p.dma(rwt, rw.re("(k p) e -> p k e", p=128))
    p.dma(rbt, rb.with_ap(rb.ap.partition_broadcast(128)))
    p.dma(b1t, b1)
    p.dma(b2t, b2)
    p.dma(cs, cvec)
    p.dma(modbt, modb)
    p.act(cs, cs, AF.Silu)

    banks = [p.psum([128, 512], F32, "bank%d" % i) for i in range(8)]
    wst = [p.sbuf([128, 8, 256], F32, "wst%d" % i) for i in range(3)]
    wsti = [0]

    def next_wst():
        t = wst[wsti[0] % 3]
        wsti[0] += 1
        return t

    modw_v = modw.re("(k p) n -> p k n", p=128)
    pm = banks[7]
    for q in range(16):
        st = next_wst()
        p.dma(st, modw_v[:, :, q * 256:(q + 1) * 256])
        for ii in range(2):
            idx = q * 2 + ii
            for k in range(8):
                p.mm(pm[:, idx * 2:idx * 2 + 2], st[:, k, ii * 128:(ii + 1) * 128], cs[:, k, :],
                     start=(k == 0), stop=(k == 7))
    mb = modbt[:, 16:48]
    p.tt(modT, pm[:, 0:64].re("p (a b) -> p a b", b=2),
         mb.with_ap(mb.ap.unsqueeze(2).to_broadcast([128, 32, 2])), ALU.add)
    p.ts(modT[:, 16:24, :], modT[:, 16:24, :], 1.0, ALU.add)

    def mvec(j, i, w):
        return modT[:, j * 8 + i, w:w + 1]

    wout_v = wout.re("(k p) n -> p k n", p=128)
    for q in range(4):
        st = next_wst()
        p.dma(st, wout_v[:, :, q * 256:(q + 1) * 256])
        p.copy(woutb[:, :, q * 256:(q + 1) * 256], st, eng="act")

    PT = 1024
    h2b, _ = p.split(p.sbuf([128, 8, PT], BF16, "h2b"), 8)
    acc, _ = p.split(p.sbuf([128, 8, PT], F32, "acc"), 8)
    gb, _ = p.split(p.sbuf([128, 8, PT], BF16, "gb"), 8)
    b1p1 = p.sbuf([128, NE * 16], F32, "b1p1")
    p.ts(b1p1, b1t, 1.0, ALU.add, eng="pool")
    gateT = p.sbuf([NE, PT], F32, "gateT")
    gbc = [p.sbuf([128, 512], F32, "gbc%d" % i) for i in range(2)]
    x1c = p.sbuf([128, 8, 512], F32, "x1c")
    ymst = [p.sbuf([128, 512], F32, "ymst%d" % i) for i in range(3)]
    ymb = p.sbuf([128, 8, 512], BF16, "ymb")
    h2f = [p.sbuf([128, 512], F32, "h2f%d" % i) for i in range(3)]
    sqt = [p.sbuf([128, 512], BF16, "sq%d" % i) for i in range(3)]
    rstd = p.sbuf([128, 512], F32, "rstd")
    rtmp = p.sbuf([128, 512], F32, "rtmp")
    lgs = p.sbuf([128, 4, NE], F32, "lgs")
    m8 = p.sbuf([128, 4, 8], F32, "m8")
    negm = p.sbuf([128, 4, 1], F32, "negm")
    msk = p.sbuf([128, 4, NE], F32, "msk")
    exs = p.sbuf([128, 4, NE], F32, "exs")
    ssum = p.sbuf([128, 4, 1], F32, "ssum")
    w1b = [p.sbuf([128, 8, 256], BF16, "w1b%d" % i) for i in range(3)]
    w2b = [p.sbuf([128, 8, 128], BF16, "w2b%d" % i) for i in range(3)]
    sw = [[p.sbuf([128, 512], F32, "sw%d_%d" % (a, b)) for b in range(4)] for a in range(2)]
    w2i = [0]
    w1i = [0]
    swi = [0]
    dmaq = [0]

    def q2():
        dmaq[0] += 1
        return "sp" if dmaq[0] % 2 else "pool"

    passes = []
    for off in range(0, n_lat, PT):
        passes.append((0, off, min(PT, n_lat - off)))
    if n_ctx:
        passes.append((1, 0, n_ctx))

    for (w, poff, pn) in passes:
        src_x = (xT if w == 0 else xcT).re("(k p) t -> p k t", p=128)
        src_y = (ymT if w == 0 else ymcT).re("(k p) t -> p k t", p=128)
        dst_x = (xoT if w == 0 else xocT).re("(k p) t -> p k t", p=128)
        chunks = [(c0, min(512, pn - c0)) for c0 in range(0, pn, 512)]
        for (c0, n) in chunks:
            t0 = poff + c0
            p.dma(x1c[:, :, :n], src_x[:, :, t0:t0 + n])
            for k in range(8):
                st = ymst[k % 3]
                p.dma(st[:, :n], src_y[:, k, t0:t0 + n], q="pool")
                p.copy(ymb[:, k, :n], st[:, :n], eng="act")
            for i in range(8):
                py = banks[4 + i % 2]
                for k in range(8):
                    p.mm(py[:, :n], woutb[:, k, i * 128:(i + 1) * 128], ymb[:, k, :n], start=(k == 0), stop=(k == 7))
                p.stt(x1c[:, i, :n], py[:, :n], mvec(0, i, w), x1c[:, i, :n], ALU.mult, ALU.add)
            p.dma(dst_x[:, :, t0:t0 + n], x1c[:, :, :n])
            pss = banks[6]
            for i in range(8):
                sq = sqt[i % 3]
                p.act(sq[:, :n], x1c[:, i, :n], AF.Square)
                p.mm(pss[:, :n], onesb, sq[:, :n], start=(i == 0), stop=(i == 7))
            p.act(rtmp[:, :n], pss[:, :n], AF.Sqrt, bias=epst, scale=1.0 / D)
            p.recip(rstd[:, :n], rtmp[:, :n])
            ntt = n // 128
            plg = banks[7]
            for i in range(8):
                hf = h2f[i % 3]
                p.tt(hf[:, :n], x1c[:, i, :n], rstd[:, :n], ALU.mult)
                p.ts(hf[:, :n], hf[:, :n], mvec(2, i, w), ALU.mult, mvec(1, i, w), ALU.add, eng="pool")
                p.copy(h2b[i][:, c0:c0 + n], hf[:, :n], eng="act")
                for tt_ in range(ntt):
                    p.mm(plg[:, tt_ * NE:(tt_ + 1) * NE], hf[:, tt_ * 128:(tt_ + 1) * 128], rwt[:, i, :],
                         start=(i == 0 and tt_ == 0), stop=(i == 7), skip_group_check=True)
            L = lgs[:, :ntt, :]
            p.tt(L, plg[:, :ntt * NE].re("p (a b) -> p a b", b=NE),
                 rbt.with_ap(rbt.ap.unsqueeze(1).to_broadcast([128, ntt, NE])), ALU.add)
            for tt_ in range(ntt):
                mo, li = m8[:, tt_, :], lgs[:, tt_, :]
                p.op("dve", (lambda o, i_: (lambda e: e.max(o, i_)))(mo.ap, li.ap), [mo], [li])
            p.ts(negm[:, :ntt, :], m8[:, :ntt, 0:1], -1.0, ALU.mult)
            th = m8[:, :ntt, 3:4]
            p.tt(msk[:, :ntt, :], L, th.with_ap(th.ap.to_broadcast([128, ntt, NE])), ALU.is_ge)
            for tt_ in range(ntt):
                p.act(exs[:, tt_, :], lgs[:, tt_, :], AF.Exp, bias=negm[:, tt_, :])
            p.tt(exs[:, :ntt, :], exs[:, :ntt, :], msk[:, :ntt, :], ALU.mult)
            p.reduce(ssum[:, :ntt, :], exs[:, :ntt, :], ALU.add)
            p.recip(ssum[:, :ntt, :], ssum[:, :ntt, :])
            sb_ = ssum[:, :ntt, :]
            p.tt(exs[:, :ntt, :], exs[:, :ntt, :], sb_.with_ap(sb_.ap.to_broadcast([128, ntt, NE])), ALU.mult)
            ptr = banks[5]
            for tt_ in range(ntt):
                p.tr(ptr[0:NE, tt_ * 128:(tt_ + 1) * 128], exs[:, tt_, :], ident)
            p.copy(gateT[:, c0:c0 + n], ptr[0:NE, :n], eng="act")
        for i in range(8):
            for (c0, n) in chunks:
                pb = banks[6 + i % 2]
                p.mm(pb[:, :n], b2t[:, i * 128:(i + 1) * 128], gateT[:, c0:c0 + n])
                p.copy(acc[i][:, c0:c0 + n], pb[:, :n], eng="dve")
        for e in range(n_exp):
            for ci, (c0, n) in enumerate(chunks):
                pg = banks[6 + ci % 2]
                oh = ident[0:NE, e:e + 1]
                p.mm(pg[:, :n], oh.with_ap(oh.ap.to_broadcast([NE, 128])), gateT[:, c0:c0 + n])
                p.copy(gbc[ci][:, :n], pg[:, :n], eng="act")
            w1v = w1[e].re("(k p) (two j c) -> p k two j c", p=128, two=2, c=128)
            for j in range(8):
                st = next_wst()
                p.dma(st[:, :, 0:128], w1v[:, :, 0, j, :], q="sp")
                p.dma(st[:, :, 128:256], w1v[:, :, 1, j, :], q="pool")
                wb = w1b[w1i[0] % 3]
                w1i[0] += 1
                p.copy(wb, st, eng="act")
                bg = b1t[:, e * 16 + j:e * 16 + j + 1]
                bl = b1p1[:, e * 16 + 8 + j:e * 16 + 8 + j + 1]
                for ci, (c0, n) in enumerate(chunks):
                    pG = banks[0 + (j * 2 + ci) % 2]
                    pL = banks[2 + (j * 2 + ci) % 2]
                    for k in range(8):
                        p.mm(pG[:, :n], wb[:, k, 0:128], h2b[k][:, c0:c0 + n], start=(k == 0), stop=(k == 7))
                    for k in range(8):
                        p.mm(pL[:, :n], wb[:, k, 128:256], h2b[k][:, c0:c0 + n], start=(k == 0), stop=(k == 7))
                    s = sw[swi[0] % 2]
                    swi[0] += 1
                    g, sg, l, t = s[0][:, :n], s[1][:, :n], s[2][:, :n], s[3][:, :n]
                    p.ts(g, pG[:, :n], bg, ALU.add, 7.0, ALU.min)
                    p.act(sg, g, AF.Sigmoid, scale=1.702)
                    p.ts(l, pL[:, :n], bl, ALU.add)
                    p.ts(l, l, 8.0, ALU.min, -6.0, ALU.max, eng="pool")
                    p.tt(t, g, sg, ALU.mult, eng="pool")
                    p.tt(t, t, l, ALU.mult, eng="pool")
                    p.tt(gb[j][:, c0:c0 + n], t, gbc[ci][:, :n], ALU.mult)
            w2v = w2[e].re("(k p) n -> p k n", p=128)
            for i in range(8):
                st = next_wst()
                p.dma(st[:, :, 0:128], w2v[:, :, i * 128:(i + 1) * 128], q=q2())
                wb2 = w2b[w2i[0] % 3]
                w2i[0] += 1
                p.copy(wb2, st[:, :, 0:128], eng="act")
                for ci, (c0, n) in enumerate(chunks):
                    pO = banks[4 + (i * 2 + ci) % 2]
                    for k in range(8):
                        p.mm(pO[:, :n], wb2[:, k, :], gb[k][:, c0:c0 + n], start=(k == 0), stop=(k == 7))
                    p.tt(acc[i][:, c0:c0 + n], acc[i][:, c0:c0 + n], pO[:, :n], ALU.add)
        for (c0, n) in chunks:
            t0 = poff + c0
            p.dma(x1c[:, :, :n], dst_x[:, :, t0:t0 + n])
            for i in range(8):
                p.stt(x1c[:, i, :n], acc[i][:, c0:c0 + n], mvec(3, i, w), x1c[:, i, :n], ALU.mult, ALU.add)
            p.dma(dst_x[:, :, t0:t0 + n], x1c[:, :, :n])
    outs = [xoT] + ([xocT] if n_ctx else [])
    p.finish(outs)
    return nc


def ffn_host_inputs(layer, core, inp, xT_b, ymT_b, xcT_b=None, ymcT_b=None, n_lat=4096):
    b, s = core // 2, core % 2
    m = {}
    m["xT"] = np.ascontiguousarray(xT_b[:, s * n_lat:(s + 1) * n_lat])
    m["ymT"] = np.ascontiguousarray(ymT_b[:, s * n_lat:(s + 1) * n_lat])
    if xcT_b is not None:
        m["xcT"] = np.ascontiguousarray(xcT_b)
        m["ymcT"] = np.ascontiguousarray(ymcT_b)
    cv = np.stack([inp["c"][b], inp["c_ctx"]], axis=-1)
    m["cvec"] = np.ascontiguousarray(cv.reshape(8, 128, 2).transpose(1, 0, 2))
    m["modw"] = np.ascontiguousarray(inp["mod_w"][layer][:, 2 * D:])
    m["modb"] = np.ascontiguousarray(inp["mod_b"][layer].reshape(48, 128).T)
    m["wout"] = inp["ab_w_out"][0] if layer == 0 else inp["cd_w_out"][0]
    m["rw"] = inp["router_w"][layer]
    m["rb"] = inp["router_b"][layer].reshape(1, NE)
    perm = np.concatenate([np.arange(0, 2 * D, 2), np.arange(1, 2 * D, 2)])
    m["w1"] = inp["_w1p"][layer]
    b1p = inp["moe_b1"][layer][:, perm]
    m["b1"] = np.ascontiguousarray(b1p.reshape(NE, 16, 128).transpose(2, 0, 1).reshape(128, NE * 16))
    m["w2"] = inp["moe_w2"][layer]
    m["b2"] = inp["moe_b2"][layer]
    m["ident"] = np.eye(128, dtype=np.float32)
    return m


D = 1024
C = 64
TB = 256
NCB = TB // C
RMS_EPS = 1e-6
LNX_EPS = 1e-5 * 64


def mk_masks():
    idx = np.arange(C)
    sf = (idx[:, None] < idx[None, :]).astype(np.float32)
    inf = (idx[:, None] <= idx[None, :]).astype(np.float32)
    sr, inr = sf.T.copy(), inf.T.copy()
    mA = np.zeros((C, 4, 128), np.float32)
    mNT = np.zeros((C, 4, C), np.float32)
    for ui in range(2):
        s_, i_ = (sf, inf) if ui == 0 else (sr, inr)
        for h in range(2):
            m = ui * 2 + h
            mA[:, m, :C] = s_
            mA[:, m, C:] = i_
            mNT[:, m, :] = -s_.T
    cm = np.ones((128, TB), np.float32)
    cm[:, ::C] = 0.0
    bo = np.zeros((128, 128), np.float32)
    bo[:64, :64] = 1.0
    bo[64:, 64:] = 1.0
    return dict(maskA=mA, maskBn=-mA, maskNT=mNT, cmask=cm, bones=bo, ident=np.eye(128, dtype=np.float32))


class Unit:
    pass


DBG_LEVEL = [99]


def chunk_engine(p, units_groups, nblk, prep_fn, banks, consts, rwkv, tag):
    ident, maskA, maskBn, maskNT = consts["ident"], consts["maskA"], consts["maskBn"], consts["maskNT"]
    ident4 = ident[0:64, 0:64]
    ident4 = ident4.with_ap(ident4.ap.unsqueeze(1).to_broadcast([64, 4, 64]))

    def sb(shape, name, n=2):
        return [p.sbuf(shape, F32, "%s_%s%d" % (tag, name, i)) for i in range(n)]

    def group_gen(gi, group):
        bX, bY, bZ = banks[3 * gi], banks[3 * gi + 1], banks[3 * gi + 2]

        def reg(bank, lo, hi, parts=64):
            return V(bank.ap[0:parts, lo:hi], Buf())
        PA = reg(bX, 0, 512).re("p (m c) -> p m c", m=4)
        PB = reg(bY, 0, 512).re("p (m c) -> p m c", m=4)
        PC = reg(bZ, 0, 256).re("p (m c) -> p m c", m=4)
        P1 = reg(bZ, 256, 512).re("p (m c) -> p m c", m=4)
        P2 = reg(bX, 0, 256).re("p (m c) -> p m c", m=4)
        P3 = reg(bX, 256, 512).re("p (m c) -> p m c", m=4)
        PT = reg(bY, 0, 512).re("p (s c) -> p s c", s=4)
        PX = reg(bZ, 0, 256).re("p (m c) -> p m c", m=4)
        PW = reg(bY, 0, 512).re("p (m c) -> p m c", m=4)
        PG = reg(bX, 0, 256, 128).re("p (u c) -> p u c", u=2)
        PR = reg(bX, 256, 384, 128).re("p (u c) -> p u c", u=2)
        PYa = reg(bX, 384, 512, 128).re("p (u c) -> p u c", u=2)
        PYb = reg(bZ, 256, 384, 128).re("p (u c) -> p u c", u=2)
        PS = reg(bZ, 384, 512, 128).re("p (u c) -> p u c", u=2)
        for r_ in (PA, P2, P3, PG, PR, PYa):
            r_.buf = bX.buf
        for r_ in (PB, PT, PW):
            r_.buf = bY.buf
        for r_ in (PC, P1, PX, PYb, PS):
            r_.buf = bZ.buf
        g = "%s_g%d" % (tag, gi)
        A_sb = sb([64, 4, 128], g + "A")
        B_sb = sb([64, 4, 128], g + "B")
        Np = sb([64, 4, 64], g + "N", 3)
        NTp = sb([64, 4, 64], g + "NT", 3)
        Acc = sb([64, 4, 64], g + "Acc", 3)
        TM = sb([64, 2, 4, 128], g + "TM")
        KX = sb([64, 4, 128], g + "KX")
        WU = sb([64, 4, 128], g + "WU")
        GH = sb([128, 2, 128], g + "GH")
        RyT = sb([128, 2, 64], g + "Ry")
        Y0 = sb([128, 2, 64], g + "Y0")
        tmpS = sb([128, 2, 64], g + "tS")
        it = [0]
        for bi in range(nblk):
            st = [prep_fn(u, u.order[bi]) for u in group]
            yield
            for jj in range(NCB if DBG_LEVEL[0] >= 2 else 0):
                k = it[0] % 2
                it[0] += 1
                js = [jj, NCB - 1 - jj]
                cs = [slice(js[ui] * C, (js[ui] + 1) * C) for ui in range(2)]
                a_, b_, tm, kx, wu, gh, ry, y0, ts_ = A_sb[k], B_sb[k], TM[k], KX[k], WU[k], GH[k], RyT[k], Y0[k], tmpS[k]
                for ui in range(2):
                    s_ = st[ui]
                    for h in range(2):
                        m = ui * 2 + h
                        ps = slice(64 * h, 64 * h + 64)
                        if rwkv:
                            p.mm(PA[:, m, 0:64], s_["kp"][ps, cs[ui]], s_["kh"][ps, cs[ui]])
                        p.mm(PA[:, m, 64:128], s_["kp"][ps, cs[ui]], s_["Rh"][ps, cs[ui]])
                        if rwkv:
                            p.mm(PB[:, m, 0:64], s_["ap"][ps, cs[ui]], s_["kh"][ps, cs[ui]])
                            p.mm(PB[:, m, 64:128], s_["ap"][ps, cs[ui]], s_["Rh"][ps, cs[ui]])
                            p.mm(PC[:, m, :], s_["kh"][ps, cs[ui]], s_["ap"][ps, cs[ui]])
                yield
                if DBG_LEVEL[0] < 3:
                    p.tt(a_, PA, maskA, ALU.mult)
                    continue
                if rwkv:
                    p.tt(a_, PA, maskA, ALU.mult)
                    p.tt(b_, PB, maskBn, ALU.mult)
                    n0, nt0 = Np[0], NTp[0]
                    p.tt(nt0, PC, maskNT, ALU.mult)
                    p.copy(n0, b_[:, :, 0:64], eng="pool")
                    acc = Acc[0]
                    p.tt(acc, b_[:, :, 0:64], ident4, ALU.add, eng="pool")
                else:
                    p.tt(a_[:, :, 64:128], PA[:, :, 64:128], maskA[:, :, 64:128], ALU.mult)
                srcs = ["kh", "V", "Kpp", "nApp"] if rwkv else ["V", "Kpp"]
                for ui in range(2):
                    for si, sn in enumerate(srcs):
                        p.tr(PT[:, si, :], st[ui][sn][:, cs[ui]], ident)
                    p.copy(tm[:, ui, 0:len(srcs), :], PT[:, 0:len(srcs), :], eng="act")
                    if rwkv:
                        p.copy(kx[:, ui * 2:ui * 2 + 2, 0:64], tm[:, ui, 0, :].re("p (h c) -> p h c", h=2), eng="pool")
                yield
                if DBG_LEVEL[0] < 4:
                    continue
                if rwkv:
                    ai = 0
                    for lv in range(5):
                        ni, no = lv % 3, (lv + 1) % 3
                        last = lv == 4
                        for m in range(4):
                            if not last:
                                p.mm(P1[:, m, :], NTp[ni][:, m, :], Np[ni][:, m, :])
                            p.mm(P2[:, m, :], Np[ni][:, m, :], NTp[ni][:, m, :])
                        yield
                        if not last:
                            p.copy(Np[no], P1, eng="act")
                        p.copy(NTp[no], P2, eng="dve")
                        for m in range(4):
                            p.mm(P3[:, m, :], NTp[no][:, m, :], Acc[ai][:, m, :])
                        yield
                        an = (ai + 1) % 3
                        p.tt(Acc[an], Acc[ai], P3, ALU.add)
                        ai = an
                    TT_ = Acc[ai]
                    for ui in range(2):
                        for h in range(2):
                            m = ui * 2 + h
                            p.mm(PX[:, m, :], a_[:, m, 0:64], tm[:, ui, 1, 64 * h:64 * h + 64])
                    yield
                    p.copy(kx[:, :, 64:128], PX, eng="act")
                    for m in range(4):
                        p.mm(PW[:, m, :], TT_[:, m, :], kx[:, m, :])
                    yield
                    p.copy(wu, PW, eng="act")
                if DBG_LEVEL[0] < 5:
                    continue
                vi, ki, ni_ = (1, 2, 3) if rwkv else (0, 1, None)
                for ui in range(2):
                    for h in range(2):
                        m = ui * 2 + h
                        hs = slice(64 * h, 64 * h + 64)
                        if rwkv:
                            p.mm(PG[hs, ui, 0:64], wu[:, m, 0:64], tm[:, ui, ni_, hs])
                        p.mm(PG[hs, ui, 64:128], tm[:, ui, ki, hs], tm[:, ui, vi, hs], start=True, stop=not rwkv)
                        if rwkv:
                            p.mm(PG[hs, ui, 64:128], tm[:, ui, ni_, hs], wu[:, m, 64:128], start=False, stop=True)
                            if DBG_LEVEL[0] >= 5.2:
                                p.mm(PR[hs, ui, :], wu[:, m, 0:64], b_[:, m, 64:128])
                        if DBG_LEVEL[0] >= 5.3:
                            p.mm(PYa[hs, ui, :], tm[:, ui, vi, hs], a_[:, m, 64:128], start=True, stop=not rwkv)
                            if rwkv:
                                p.mm(PYa[hs, ui, :], wu[:, m, 64:128], b_[:, m, 64:128], start=False, stop=True)
                yield
                if rwkv:
                    p.copy(gh, PG, eng="act")
                    if DBG_LEVEL[0] >= 5.25:
                        for ui in range(2):
                            p.tt(ry[:, ui, :], PR[:, ui, :], st[ui]["Rh"][:, cs[ui]], ALU.add)
                else:
                    p.copy(gh[:, :, 64:128], PG[:, :, 64:128], eng="act")
                if DBG_LEVEL[0] >= 5.3:
                    p.copy(y0, PYa, eng="pool" if False else "dve")
                if DBG_LEVEL[0] < 6:
                    continue
                for ui in range(2):
                    u = group[ui]
                    for h in range(2):
                        hs = slice(64 * h, 64 * h + 64)
                        rhs_r = ry[hs, ui, :] if rwkv else st[ui]["Rh"][hs, cs[ui]]
                        p.mm(PYb[hs, ui, :], u.S[hs, :], rhs_r)
                        if rwkv:
                            p.mm(PS[hs, ui, :], gh[hs, ui, 0:64], u.S[hs, :])
                yield
                for ui in range(2):
                    u = group[ui]
                    p.tt(u.yout[:, cs[ui]], y0[:, ui, :], PYb[:, ui, :], ALU.add)
                    eb = st[ui]["eB"][:, js[ui]:js[ui] + 1]
                    if rwkv:
                        p.tt(ts_[:, ui, :], PS[:, ui, :], gh[:, ui, 64:128], ALU.add)
                        p.stt(u.S, u.S, eb, ts_[:, ui, :], ALU.mult, ALU.add)
                    else:
                        p.stt(u.S, u.S, eb, gh[:, ui, 64:128], ALU.mult, ALU.add)
                yield
            for ui in range(2):
                u = group[ui]
                t0 = u.order[bi] * TB
                p.dma(u.ydst[:, t0:t0 + TB], u.yout, q="pool")
            yield

    gens = [group_gen(gi, g_) for gi, g_ in enumerate(units_groups)]
    alive = list(gens)
    while alive:
        nxt = []
        for g_ in alive:
            try:
                next(g_)
                nxt.append(g_)
            except StopIteration:
                pass
        alive = nxt


class Ring:
    def __init__(self, p, shape, dt, name, n=2):
        self.t = [p.sbuf(shape, dt, "%s%d" % (name, i)) for i in range(n)]
        self.i = 0

    def get(self):
        t = self.t[self.i % len(self.t)]
        self.i += 1
        return t


def emit_mod(p, banks, cs, modw, modbt, n_chunks, wst_ring, out_modT):
    modw_v = modw.re("(k p) n -> p k n", p=128)
    pm = banks[7]
    nq = n_chunks * 4
    for q in range(nq):
        st = wst_ring.get()
        p.dma(st, modw_v[:, :, q * 256:(q + 1) * 256], q="sp" if q % 2 == 0 else "pool")
        for ii in range(2):
            idx = q * 2 + ii
            for k in range(8):
                p.mm(pm[:, idx * 2:idx * 2 + 2], st[:, k, ii * 128:(ii + 1) * 128], cs[:, k, :],
                     start=(k == 0), stop=(k == 7))
    n = n_chunks * 8
    p.tt(out_modT, pm[:, 0:2 * n].re("p (a b) -> p a b", b=2),
         modbt.with_ap(modbt.ap.unsqueeze(2).to_broadcast([128, n, 2])), ALU.add)


def build_mix0(nr, nh, n_lat=8192, n_ctx=256, debug=False, stop=99):
    nc = bass.Bass("TRN2", target_bir_lowering=False)
    p = Prog(nc)
    TT = n_ctx + n_lat
    nblk = TT // TB
    nct = (3 * nr + 3) + 5 * nh
    inp = lambda name, shape: p.dram(name, shape, F32, kind="ExternalInput")
    xT = inp("xT", [D, n_lat]); xcT = inp("xcT", [D, n_ctx])
    cvec = inp("cvec", [128, 8, 2]); modw = inp("modw", [D, 2 * D]); modb = inp("modb", [128, 16])
    win = inp("win", [D, nct * 128])
    taps = inp("taps", [128, (3 * nr + 3) * 3])
    rw0 = inp("rw0", [128, nr * 2]); ra0 = inp("ra0", [128, nr * 2])
    rw2 = inp("rw2", [128, nr * 128]); ra2 = inp("ra2", [128, nr * 128]); rg2 = inp("rg2", [128, nr * 128])
    rvec = inp("rvec", [128, nr * 5]); hvec = inp("hvec", [128, nh * 3])
    cd = {k: inp("c_" + k, list(v.shape)) for k, v in mk_masks().items()}
    ymix = p.dram("ymix", [(nr + nh) * 128, TT], F32, kind="ExternalOutput")
    uT = p.dram("uT", [nct * 128, TT], F32, kind="ExternalOutput" if debug == 1 else "Internal")
    yd = p.dram("yd", [(nr + nh) * 2 * 128, TT], F32, kind="ExternalOutput" if debug == 2 else "Internal")
    bon = p.dram("bon", [nr * 2 * 128, TT], F32, kind="ExternalOutput" if debug == 2 else "Internal")

    banks = [p.psum([128, 512], F32, "bank%d" % i) for i in range(8)]
    consts = {}
    for k, v in cd.items():
        shp = list(v.ap.shape)
        consts[k] = p.sbuf(shp, F32, "k_" + k)
        p.dma(consts[k], v)
    ident, bones, cmask = consts["ident"], consts["bones"], consts["cmask"]
    consts_eps = {"lnx": p.sbuf([128, 1], F32, "epsl"), "rms": p.sbuf([128, 1], F32, "epsr")}
    p.memset(consts_eps["lnx"], LNX_EPS)
    p.memset(consts_eps["rms"], RMS_EPS)

    def sm(name, src, shape):
        t = p.sbuf(shape, F32, name)
        p.dma(t, src if len(shape) == 2 else src.re("p (a b) -> p a b", b=shape[2]))
        return t
    tapst = sm("tapst", taps, [128, 3 * nr + 3, 3])
    rw0t = sm("rw0t", rw0, [128, nr, 2]); ra0t = sm("ra0t", ra0, [128, nr, 2])
    rw2t = sm("rw2t", rw2, [128, nr, 128]); ra2t = sm("ra2t", ra2, [128, nr, 128]); rg2t = sm("rg2t", rg2, [128, nr, 128])
    rvt = sm("rvt", rvec, [128, nr, 5]); hvt = sm("hvt", hvec, [128, nh, 3])
    lbt = p.sbuf([128, nh, 2], F32, "lbt")
    p.tt(lbt[:, :, 0:1], hvt[:, :, 0:1], hvt[:, :, 1:2], ALU.subtract)
    p.act(lbt[:, :, 0:1], lbt[:, :, 0:1], AF.Sigmoid)
    p.ts(lbt[:, :, 1:2], lbt[:, :, 0:1], -1.0, ALU.mult, 1.0, ALU.add)

    with p.phase():
        cs = p.sbuf([128, 8, 2], F32, "cs"); modbt = p.sbuf([128, 16], F32, "modbt")
        modT = p.sbuf([128, 16, 2], F32, "modT")
        epst = p.sbuf([128, 1], F32, "epst"); onesb = p.sbuf([128, 128], BF16, "onesb")
        p.memset(epst, RMS_EPS); p.memset(onesb, 1.0)
        p.dma(cs, cvec); p.dma(modbt, modb)
        p.act(cs, cs, AF.Silu)
        wst = Ring(p, [128, 8, 256], F32, "wst", 3)
        emit_mod(p, banks, cs, modw, modbt, 2, wst, modT)
        p.ts(modT[:, 8:16, :], modT[:, 8:16, :], 1.0, ALU.add)
        winb = p.sbuf([128, 8, nct * 128], BF16, "winb")
        win_v = win.re("(k p) n -> p k n", p=128)
        for q in range(0, nct * 128, 256):
            n = min(256, nct * 128 - q)
            st = wst.get()
            p.dma(st[:, :, :n], win_v[:, :, q:q + n])
            p.copy(winb[:, :, q:q + n], st[:, :, :n], eng="act")
        xc = Ring(p, [128, 8, 512], F32, "xc", 2)
        hb = Ring(p, [128, 8, 512], BF16, "hb", 2)
        sqr = Ring(p, [128, 512], BF16, "sq", 3)
        hfr = Ring(p, [128, 512], F32, "hf", 3)
        ust = Ring(p, [128, 512], F32, "ust", 4)
        rstd = p.sbuf([128, 512], F32, "rstd"); rtmp = p.sbuf([128, 512], F32, "rtmp")
        chunks = [(1, 0, n_ctx)] + [(0, o, 512) for o in range(0, n_lat, 512)]
        for (w, o, n) in chunks:
            src = (xT if w == 0 else xcT).re("(k p) t -> p k t", p=128)
            x_ = xc.get()
            p.dma(x_[:, :, :n], src[:, :, o:o + n])
            pss = banks[6]
            for i in range(8):
                sq = sqr.get()
                p.act(sq[:, :n], x_[:, i, :n], AF.Square)
                p.mm(pss[:, :n], onesb, sq[:, :n], start=(i == 0), stop=(i == 7))
            p.act(rtmp[:, :n], pss[:, :n], AF.Sqrt, bias=epst, scale=1.0 / D)
            p.recip(rstd[:, :n], rtmp[:, :n])
            h_ = hb.get()
            for i in range(8):
                hf = hfr.get()
                p.tt(hf[:, :n], x_[:, i, :n], rstd[:, :n], ALU.mult, eng="dve" if i % 2 else "pool")
                p.ts(h_[:, i, :n], hf[:, :n], modT[:, 8 + i, w:w + 1], ALU.mult, modT[:, i, w:w + 1], ALU.add,
                     eng="pool" if i % 2 else "dve")
            tdst = (0 if w == 1 else n_ctx) + o
            for ct in range(nct):
                pu = banks[ct % 4]
                for k in range(8):
                    p.mm(pu[:, :n], winb[:, k, ct * 128:(ct + 1) * 128], h_[:, k, :n], start=(k == 0), stop=(k == 7))
                us = ust.get()
                p.copy(us[:, :n], pu[:, :n], eng="act" if ct % 2 else "dve")
                p.dma(uT[ct * 128:(ct + 1) * 128, tdst:tdst + n], us[:, :n], q="sp" if ct % 2 else "pool")

    if stop <= 0:
        p.finish(); return nc
    order_f = list(range(nblk))
    order_b = [0] + list(range(nblk - 1, 0, -1))
    nctx_blk = n_ctx // TB

    def seg_edges(tb):
        left0 = (tb == 0) or (tb == nctx_blk)
        right0 = (tb == nctx_blk - 1) or (tb == nblk - 1)
        return left0, right0

    def load_halo(dst, row0, prt, tb, q="sp"):
        t0 = tb * TB
        l0, r0 = seg_edges(tb)
        a = 1 if l0 else 0
        b = TB + 1 if r0 else TB + 2
        if l0:
            p.memset(dst[prt, 0:1], 0.0, eng="pool")
        if r0:
            p.memset(dst[prt, TB + 1:TB + 2], 0.0, eng="pool")
        p.dma(dst[prt, a:b], uT[row0 + prt.start:row0 + prt.stop, t0 - 1 + a:t0 - 1 + b], q=q)

    def conv3(out, uh, prt, ctile, eng="dve"):
        tp = tapst[prt, ctile, :]
        p.ts(out, uh[prt, 1:TB + 1], tp[:, 1:2], ALU.mult, eng=eng)
        p.stt(out, uh[prt, 0:TB], tp[:, 0:1], out, ALU.mult, ALU.add)
        p.stt(out, uh[prt, 2:TB + 2], tp[:, 2:3], out, ALU.mult, ALU.add)

    ALLP = slice(0, 128)

    with p.phase():
        units = []
        for ihp in range(nr):
            for d in range(2):
                u = Unit()
                u.ihp, u.d = ihp, d
                u.order = order_f if d == 0 else order_b
                u.S = p.sbuf([128, 64], F32, "S%d_%d" % (ihp, d))
                p.memset(u.S, 0.0)
                u.yout = p.sbuf([128, TB], F32, "yo%d_%d" % (ihp, d))
                u.ydst = yd[(ihp * 2 + d) * 128:(ihp * 2 + d + 1) * 128, :]
                u.fin = {k: Ring(p, [128, TB], F32, "f%s%d_%d" % (k, ihp, d), 2) for k in ("kh", "Rh", "kp", "ap", "Kpp", "nApp", "V")}
                u.eB = Ring(p, [128, NCB], F32, "eB%d_%d" % (ihp, d), 2)
                units.append(u)
        T = {}

        def tmp(name, shape=(128, TB), n=2):
            if name not in T:
                T[name] = Ring(p, list(shape), F32, "t_" + name, n)
            return T[name].get()

        def prep(u, tb):
            ihp, d = u.ihp, u.d
            t0 = tb * TB
            uh = {}
            for si, sn in enumerate(("r", "k", "v")):
                uh[sn] = tmp("uh" + sn, (128, TB + 2))
                load_halo(uh[sn], (si * nr + ihp) * 128, ALLP, tb, q="sp")
            dp = slice(64 * d, 64 * d + 64)
            uw = tmp("uhw", (128, TB + 2)); ua = tmp("uha", (128, TB + 2))
            load_halo(uw, (3 * nr) * 128, dp, tb, q="sp")
            load_halo(ua, (3 * nr + 1) * 128, dp, tb, q="sp")
            rc, kc = tmp("rc"), tmp("kc")
            vc = u.fin["V"].get()
            conv3(rc, uh["r"], ALLP, 0 * nr + ihp)
            conv3(kc, uh["k"], ALLP, 1 * nr + ihp, eng="pool")
            conv3(vc, uh["v"], ALLP, 2 * nr + ihp)
            wdc, adc = tmp("wdc"), tmp("adc")
            conv3(wdc[dp, :], uw, dp, 3 * nr, eng="pool")
            conv3(adc[dp, :], ua, dp, 3 * nr + 1, eng="pool")
            p.act(wdc[dp, :], wdc[dp, :], AF.Tanh)
            pw = banks[6]; pa = banks[7]
            p.mm(pw[:, 0:TB], rw2t[dp, ihp, :], wdc[dp, :])
            p.mm(pa[:, 0:TB], ra2t[dp, ihp, :], adc[dp, :])
            lw = tmp("lw"); a = tmp("a")
            p.act(lw, pw[:, 0:TB], AF.Sigmoid, bias=rw0t[:, ihp, d:d + 1])
            p.ts(lw, lw, -float(np.exp(-0.5)), ALU.mult, eng="pool")
            p.act(a, pa[:, 0:TB], AF.Sigmoid, bias=ra0t[:, ihp, d:d + 1])
            kk = tmp("kk"); sq = tmp("sq")
            p.ts(kk, kc, rvt[:, ihp, 0:1], ALU.mult)
            p.act(sq, kk, AF.Square)
            p.mm(pw[:, TB:2 * TB], bones, sq)
            nrm = tmp("nrm")
            p.act(nrm, pw[:, TB:2 * TB], AF.Sqrt)
            p.ts(nrm, nrm, 1e-12, ALU.max, eng="pool")
            p.recip(nrm, nrm)
            kap = tmp("kap"); alp = tmp("alp"); kt = tmp("kt")
            p.tt(kap, kk, nrm, ALU.mult)
            p.tt(alp, a, kap, ALU.mult, eng="pool")
            p.ts(kt, a, -1.0, ALU.add, rvt[:, ihp, 1:2], ALU.mult)
            p.stt(kt, kt, 1.0, kc, ALU.add, ALU.mult)
            pk = tmp("pk")
            p.stt(pk, rc, rvt[:, ihp, 2:3], kt, ALU.mult, ALU.mult)
            p.mm(pa[:, TB:2 * TB], bones, pk)
            bo_ = tmp("bo")
            p.tt(bo_, pa[:, TB:2 * TB], vc, ALU.mult)
            p.dma(bon[(ihp * 2 + d) * 128:(ihp * 2 + d + 1) * 128, t0:t0 + TB], bo_, q="pool")
            return finish_streams(u, d, lw, rc, kt, kap, alp, vc)

        def finish_streams(u, d, lw, rq, kt, kap, alp, vc):
            bf = tmp("bf")
            p.scan(bf, cmask, lw, 0.0, ALU.mult, ALU.add)
            bf3 = bf.re("p (n c) -> p n c", c=C)
            Bv = bf3[:, :, C - 1:C]
            if d == 0:
                binc = bf
            else:
                binc = tmp("binc")
                p.tt(binc.re("p (n c) -> p n c", c=C), Bv.with_ap(Bv.ap.to_broadcast([128, NCB, C])), bf3, ALU.subtract)
                p.tt(binc, binc, lw, ALU.add, eng="pool")
            eB = u.eB.get()
            p.act(eB, Bv.re("p n c -> p (n c)"), AF.Exp)
            eBb = eB.with_ap(eB.ap.unsqueeze(2).to_broadcast([128, NCB, C]))
            E2 = tmp("E2"); E3 = tmp("E3")
            p.act(E2, binc, AF.Exp)
            p.act(E3, binc, AF.Exp, scale=-1.0)
            out = {"V": vc, "eB": eB}
            Rh = u.fin["Rh"].get(); kp = u.fin["kp"].get(); Kpp = u.fin["Kpp"].get()
            p.tt(Rh, rq, E2, ALU.mult)
            p.tt(kp, kt, E3, ALU.mult, eng="pool")
            p.tt(Kpp.re("p (n c) -> p n c", c=C), kp.re("p (n c) -> p n c", c=C), eBb, ALU.mult)
            out.update(Rh=Rh, kp=kp, Kpp=Kpp)
            if kap is not None:
                bexc = tmp("bexc"); E1 = tmp("E1")
                p.tt(bexc, binc, lw, ALU.subtract, eng="pool")
                p.act(E1, bexc, AF.Exp)
                kh = u.fin["kh"].get(); ap_ = u.fin["ap"].get(); nApp = u.fin["nApp"].get()
                p.tt(kh, kap, E1, ALU.mult)
                p.tt(ap_, alp, E3, ALU.mult, eng="pool")
                p.stt(nApp.re("p (n c) -> p n c", c=C), ap_.re("p (n c) -> p n c", c=C), -1.0, eBb, ALU.mult, ALU.mult)
                out.update(kh=kh, ap=ap_, nApp=nApp)
            return out

        groups = [[units[2 * i], units[2 * i + 1]] for i in range(nr)]
        for g0 in range(0, nr if not os.environ.get("SKIP_RW") else 0, 2):
            chunk_engine(p, groups[g0:g0 + 2], nblk, prep, banks, consts, True, "rw%d" % g0)

    if stop <= 1:
        p.finish(); return nc
    with p.phase():
        T = {}

        def tmp2(name, shape=(128, TB), n=2):
            if name not in T:
                T[name] = Ring(p, list(shape), F32, "r_" + name, n)
            return T[name].get()
        for ihp in range(nr):
            for tb in range(nblk):
                t0 = tb * TB
                yf = tmp2("yf"); yb_ = tmp2("yb"); bf_ = tmp2("bf"); bb_ = tmp2("bb")
                p.dma(yf, yd[(ihp * 2) * 128:(ihp * 2 + 1) * 128, t0:t0 + TB])
                p.dma(yb_, yd[(ihp * 2 + 1) * 128:(ihp * 2 + 2) * 128, t0:t0 + TB], q="pool")
                p.dma(bf_, bon[(ihp * 2) * 128:(ihp * 2 + 1) * 128, t0:t0 + TB])
                p.dma(bb_, bon[(ihp * 2 + 1) * 128:(ihp * 2 + 2) * 128, t0:t0 + TB], q="pool")
                ug = tmp2("ug", (128, TB + 2))
                load_halo(ug, (3 * nr + 2) * 128, ALLP, tb)
                gdc = tmp2("gdc")
                conv3(gdc, ug, ALLP, 3 * nr + 2, eng="pool")
                p.act(gdc, gdc, AF.Sigmoid)
                pg = banks[0 + tb % 2]
                p.mm(pg[:, 0:TB], rg2t[:, ihp, :], gdc)
                y = tmp2("y")
                p.tt(y, yf, yb_, ALU.add)
                pm_ = banks[2 + tb % 2]
                p.mm(pm_[:, 0:TB], bones, y)
                yc = tmp2("yc")
                p.stt(yc, pm_[:, 0:TB], -1.0 / 64, y, ALU.mult, ALU.add)
                sq = tmp2("sq")
                p.act(sq, yc, AF.Square)
                p.mm(pm_[:, TB:2 * TB], bones, sq)
                sd = tmp2("sd")
                epsl = consts_eps["lnx"]
                p.act(sd, pm_[:, TB:2 * TB], AF.Sqrt, bias=epsl, scale=1.0 / 64)
                p.recip(sd, sd)
                p.tt(yc, yc, sd, ALU.mult)
                p.ts(yc, yc, rvt[:, ihp, 3:4], ALU.mult, rvt[:, ihp, 4:5], ALU.add, eng="pool")
                p.tt(bf_, bf_, bb_, ALU.add, eng="pool")
                p.tt(yc, yc, bf_, ALU.add)
                o = tmp2("o")
                p.tt(o, yc, pg[:, 0:TB], ALU.mult)
                p.dma(ymix[ihp * 128:(ihp + 1) * 128, t0:t0 + TB], o, q="pool")

    if stop <= 2:
        p.finish(); return nc
    with p.phase():
        units = []
        for ihp in range(nh):
            for d in range(2):
                u = Unit()
                u.ihp, u.d = ihp, d
                u.order = order_f if d == 0 else order_b
                u.S = p.sbuf([128, 64], F32, "hS%d_%d" % (ihp, d))
                p.memset(u.S, 0.0)
                u.yout = p.sbuf([128, TB], F32, "hyo%d_%d" % (ihp, d))
                row = (nr * 2 + ihp * 2 + d) * 128
                u.ydst = yd[row:row + 128, :]
                u.fin = {k: Ring(p, [128, TB], F32, "hf%s%d_%d" % (k, ihp, d), 2) for k in ("Rh", "kp", "Kpp", "V")}
                u.eB = Ring(p, [128, NCB], F32, "heB%d_%d" % (ihp, d), 2)
                units.append(u)
        T = {}

        def tmp(name, shape=(128, TB), n=2):
            if name not in T:
                T[name] = Ring(p, list(shape), F32, "h_" + name, n)
            return T[name].get()
        hbase = (3 * nr + 3)

        def hprep(u, tb):
            ihp, d = u.ihp, u.d
            t0 = tb * TB
            q_ = tmp("q"); f_ = tmp("f")
            vi = u.fin["V"].get()
            p.dma(q_, uT[(hbase + 0 * nh + ihp) * 128:(hbase + 0 * nh + ihp + 1) * 128, t0:t0 + TB])
            p.dma(f_, uT[(hbase + (1 + d) * nh + ihp) * 128:(hbase + (1 + d) * nh + ihp + 1) * 128, t0:t0 + TB], q="pool")
            p.dma(vi, uT[(hbase + 3 * nh + ihp) * 128:(hbase + 3 * nh + ihp + 1) * 128, t0:t0 + TB])
            qs = tmp("qs")
            p.act(qs, q_, AF.Silu)
            fg = tmp("fg")
            p.act(fg, f_, AF.Sigmoid)
            p.ts(fg, fg, lbt[:, ihp, 1:2], ALU.mult, lbt[:, ihp, 0:1], ALU.add)
            kt = tmp("kt")
            p.ts(kt, fg, -1.0, ALU.mult, 1.0, ALU.add, eng="pool")
            lw = tmp("lw")
            p.act(lw, fg, AF.Ln)
            return finish_streams(u, d, lw, qs, kt, None, None, vi)

        groups = [[units[2 * i], units[2 * i + 1]] for i in range(nh)]
        for g0 in range(0, nh, 2):
            chunk_engine(p, groups[g0:g0 + 2], nblk, hprep, banks, consts, False, "hg%d" % g0)

    if stop <= 3:
        p.finish(); return nc
    with p.phase():
        T = {}
        for ihp in range(nh):
            for tb in range(nblk):
                t0 = tb * TB
                of = tmp2("of"); ob = tmp2("ob"); og = tmp2("og")
                r0 = (nr * 2 + ihp * 2) * 128
                p.dma(of, yd[r0:r0 + 128, t0:t0 + TB])
                p.dma(ob, yd[r0 + 128:r0 + 256, t0:t0 + TB], q="pool")
                p.dma(og, uT[(hbase + 4 * nh + ihp) * 128:(hbase + 4 * nh + ihp + 1) * 128, t0:t0 + TB])
                o = tmp2("o")
                p.tt(o, of, ob, ALU.add)
                sq = tmp2("sq")
                p.act(sq, o, AF.Square)
                pm_ = banks[tb % 4]
                p.mm(pm_[:, 0:TB], bones, sq)
                sd = tmp2("sd")
                p.act(sd, pm_[:, 0:TB], AF.Sqrt, bias=consts_eps["rms"], scale=1.0 / 64)
                p.recip(sd, sd)
                p.tt(o, o, sd, ALU.mult)
                p.act(og, og, AF.Silu)
                p.stt(o, o, hvt[:, ihp, 2:3], og, ALU.mult, ALU.mult)
                p.dma(ymix[(nr + ihp) * 128:(nr + ihp + 1) * 128, t0:t0 + TB], o, q="pool")
    p.finish()
    return nc


A_OFF = dict(r=0, k=512, v=1024, wd=1536, ad=1664, gd=1792)
B_OFF = dict(q=0, f0=512, f1=1024, i=1536, og=2048)


def mix0_cols(pairs_r, pairs_h):
    cols = []
    for s in ("r", "k", "v"):
        for g in pairs_r:
            cols.append(np.arange(A_OFF[s] + g * 128, A_OFF[s] + g * 128 + 128))
    for s in ("wd", "ad", "gd"):
        cols.append(np.arange(A_OFF[s], A_OFF[s] + 128))
    nrw = len(cols)
    for s in ("q", "f0", "f1", "i", "og"):
        for g in pairs_h:
            cols.append(1920 + np.arange(B_OFF[s] + g * 128, B_OFF[s] + g * 128 + 128))
    return np.concatenate(cols), nrw


def mix0_host_inputs(b, pairs_r, pairs_h, inp, xT_b, xcT_b):
    m = {}
    cols, nrw = mix0_cols(pairs_r, pairs_h)
    m["xT"] = np.ascontiguousarray(xT_b); m["xcT"] = np.ascontiguousarray(xcT_b)
    cv = np.stack([inp["c"][b], inp["c_ctx"]], axis=-1)
    m["cvec"] = np.ascontiguousarray(cv.reshape(8, 128, 2).transpose(1, 0, 2))
    m["modw"] = np.ascontiguousarray(inp["mod_w"][0][:, 0:2048])
    m["modb"] = np.ascontiguousarray(inp["mod_b"][0][0:2048].reshape(16, 128).T)
    m["win"] = np.ascontiguousarray(inp["ab_w_in"][0][:, cols])
    sh = inp["rwkv_shift"][0][:, cols[:nrw * 128]]
    m["taps"] = np.ascontiguousarray(sh.reshape(3, nrw, 128).transpose(2, 1, 0).reshape(128, nrw * 3))
    nr = len(pairs_r)
    ch = np.stack([np.arange(g * 128, g * 128 + 128) for g in pairs_r], 0)
    def pc(v):
        return v[..., ch]
    m["rw0"] = np.ascontiguousarray(inp["rwkv_w0"][0][:, ch].transpose(2, 1, 0).reshape(128, nr * 2))
    m["ra0"] = np.ascontiguousarray(inp["rwkv_a0"][0][:, ch].transpose(2, 1, 0).reshape(128, nr * 2))
    w2 = inp["rwkv_w2"][0][:, :, ch]
    m["rw2"] = np.ascontiguousarray(w2.reshape(128, nr * 128))
    a2 = inp["rwkv_a2"][0][:, :, ch]
    m["ra2"] = np.ascontiguousarray(a2.reshape(128, nr * 128))
    m["rg2"] = np.ascontiguousarray(inp["rwkv_g2"][0][:, ch].reshape(128, nr * 128))
    rk = inp["rwkv_r_k"][0].reshape(512)
    rv = np.stack([inp["rwkv_k_k"][0][ch], inp["rwkv_k_a"][0][ch], rk[ch], inp["rwkv_ln_w"][0][ch], inp["rwkv_ln_b"][0][ch]], -1)
    m["rvec"] = np.ascontiguousarray(rv.transpose(1, 0, 2).reshape(128, nr * 5))
    nh = len(pairs_h)
    chh = np.stack([np.arange(g * 128, g * 128 + 128) for g in pairs_h], 0)
    nw = np.tile(inp["hgrn_norm_w"][0], 8)
    hv = np.stack([inp["hgrn_lb_logits"][0][chh], inp["hgrn_lb_logits"][1][chh], nw[chh]], -1)
    m["hvec"] = np.ascontiguousarray(hv.transpose(1, 0, 2).reshape(128, nh * 3))
    for k, v in mk_masks().items():
        m["c_" + k] = v
    return m


GRID_W = 64
NA_ROWS, NA_COLS = 8, 16
CG = 16
NFFT = 16384
NEG = -30000.0


def na_bias_variants(rpb, n_rows):
    nblk = n_rows // 2
    H = rpb.shape[0]
    ntile = n_rows // 2
    specials = sorted(set([0, 1, nblk - 2, nblk - 1]))
    var_of = {}
    variants = []

    def build(i):
        j0 = min(max(i - 2, 0), ntile - 5)
        out = np.full((5, H, 128, 128), NEG, np.float32)
        for qi in range(128):
            qr, qc = 2 * i + qi // 64, qi % 64
            r0 = min(max(qr - NA_ROWS // 2, 0), n_rows - NA_ROWS)
            c0 = min(max(qc - NA_COLS // 2, 0), GRID_W - NA_COLS)
            for kr in range(r0, r0 + NA_ROWS):
                jt = kr // 2 - j0
                assert 0 <= jt < 5
                ro = kr - qr + NA_ROWS - 1
                for kc in range(c0, c0 + NA_COLS):
                    co = kc - qc + NA_COLS - 1
                    out[jt, :, (kr % 2) * 64 + kc, qi] = rpb[:, ro, co]
        return out, j0
    interior = 2 if nblk > 4 else None
    blocks = []
    for i in range(nblk):
        key = i if (i in specials or interior is None) else "int"
        if key not in var_of:
            var_of[key] = len(variants)
            variants.append(build(i)[0])
        j0 = min(max(i - 2, 0), ntile - 5)
        blocks.append((var_of[key], j0))
    return np.stack(variants, 0), blocks


def rope_tables(n_lat):
    t = np.arange(n_lat)
    pos = np.stack([t // GRID_W, t % GRID_W], -1).astype(np.float32)
    nf = 16
    inv = (10000.0 ** (-np.arange(nf, dtype=np.float32) / nf)).astype(np.float32)
    cosT = np.zeros((128, n_lat), np.float32)
    sinT = np.zeros((128, n_lat), np.float32)
    perm = np.zeros((128, 128), np.float32)
    for c in range(128):
        cc = c % 64
        axis, half, f = cc // 32, (cc % 32) // 16, cc % 16
        ang = pos[:, axis] * inv[f]
        cosT[c] = np.cos(ang)
        sinT[c] = np.sin(ang) * (-1.0 if half == 0 else 1.0)
        partner = c + 16 if half == 0 else c - 16
        perm[partner, c] = 1.0
    return cosT, sinT, perm


def hyena_consts(L):
    f32 = np.float32
    t = np.linspace(0.0, 1.0, L, dtype=f32)
    bands = 16
    f = np.linspace(1e-4, bands - 1, bands, dtype=f32)
    ang = (2.0 * math.pi / L) * np.arange(L, dtype=f32)[:, None] * f
    z = np.concatenate([t[:, None], np.cos(ang), -np.sin(ang)], -1).astype(f32)
    zT = np.ascontiguousarray(z.T)
    idx = (L - np.arange(L)) % L
    zTr = np.ascontiguousarray(zT[:, idx])
    tn = t.copy()
    tnr = t[idx].copy()
    tnr[0] = 1e4
    deltas = np.abs(np.linspace(math.log(1e-2) / 1.5, math.log(1e-2) / 0.3, 512, dtype=f32)).astype(f32)
    n = np.arange(128)
    Fr = np.cos(2 * np.pi * np.outer(n, n) / 128).astype(f32)
    Fi = (-np.sin(2 * np.pi * np.outer(n, n) / 128)).astype(f32)
    Tr = np.cos(2 * np.pi * np.outer(n, n) / NFFT).astype(f32)
    Ti = (-np.sin(2 * np.pi * np.outer(n, n) / NFFT)).astype(f32)
    dft = np.stack([Fr, Fi, -Fi], 0)
    return dict(zT=zT, zTr=zTr, tn=tn.reshape(1, L), tnr=tnr.reshape(1, L), deltas=deltas, dft=dft, Tr=Tr, Ti=Ti)


def build_mix1(n_hp, n_ht, n_lat=8192, n_ctx=256, stop=99, debug=0):
    nc = bass.Bass("TRN2", target_bir_lowering=False)
    p = Prog(nc)
    TT = n_ctx + n_lat
    n_rows = n_lat // GRID_W
    nqb = n_lat // 128
    nct = 3 * n_hp + 3 * n_ht
    inp = lambda name, shape: p.dram(name, shape, F32, kind="ExternalInput")
    xT = inp("xT", [D, n_lat]); xcT = inp("xcT", [D, n_ctx])
    cvec = inp("cvec", [128, 8, 2]); modw = inp("modw", [D, 2 * D]); modb = inp("modb", [128, 16])
    win = inp("win", [D, nct * 128])
    qkn = inp("qkn", [128, 2])
    _, blocks = na_bias_variants(np.zeros((2 * n_hp, 15, 31), np.float32), n_rows)
    nvar = max(b[0] for b in blocks) + 1
    biasd = inp("biasT", [128, nvar * 5 * 2 * n_hp * 128])
    cosd = inp("cosT", [128, n_lat]); sind = inp("sinT", [128, n_lat]); permd = inp("perm", [128, 128])
    identd = inp("ident", [128, 128]); bonesd = inp("bones", [128, 128])
    taps = inp("taps", [128, n_ht * 3 * 3])
    hyw1 = inp("hyw1", [33, 64]); hyv = inp("hyv", [64, 4]); hyw2 = inp("hyw2", [64, 64])
    hyw3 = inp("hyw3", [64, 4 * n_ht * 128])
    hybias = inp("hybias", [1, 2 * n_ht * 128])
    hdel = inp("hdel", [128, n_ht])
    zTd = inp("zT", [33, n_lat]); zTrd = inp("zTr", [33, n_lat]); tnd = inp("tn", [1, n_lat]); tnrd = inp("tnr", [1, n_lat])
    dftd = inp("dft", [3 * 128, 128]); Trd = inp("Tr", [128, 128]); Tid = inp("Ti", [128, 128])
    ymix = p.dram("ymix", [(n_hp + n_ht) * 128, n_lat], F32, kind="ExternalOutput")
    uT = p.dram("uT", [nct * 128, TT], F32, kind="ExternalOutput" if debug == 1 else "Internal")
    hcv = p.dram("hcv", [3 * n_ht * 128, n_lat], F32)
    filt = p.dram("filt", [4 * n_ht * 128, n_lat], F32, kind="ExternalOutput" if debug == 2 else "Internal")
    hb0d = p.dram("hb0d", [2 * n_ht * 128, 1], F32)

    banks = [p.psum([128, 512], F32, "bank%d" % i) for i in range(8)]
    ident = p.sbuf([128, 128], F32, "ident"); bones = p.sbuf([128, 128], F32, "bones")
    p.dma(ident, identd); p.dma(bones, bonesd)
    epsr = p.sbuf([128, 1], F32, "epsr"); p.memset(epsr, RMS_EPS)

    with p.phase():
        cs = p.sbuf([128, 8, 2], F32, "cs"); modbt = p.sbuf([128, 16], F32, "modbt")
        modT = p.sbuf([128, 16, 2], F32, "modT")
        onesb = p.sbuf([128, 128], BF16, "onesb")
        p.memset(onesb, 1.0)
        p.dma(cs, cvec); p.dma(modbt, modb)
        p.act(cs, cs, AF.Silu)
        wst = Ring(p, [128, 8, 256], F32, "wst", 3)
        emit_mod(p, banks, cs, modw, modbt, 2, wst, modT)
        p.ts(modT[:, 8:16, :], modT[:, 8:16, :], 1.0, ALU.add)
        winb = p.sbuf([128, 8, nct * 128], BF16, "winb")
        win_v = win.re("(k p) n -> p k n", p=128)
        for q in range(0, nct * 128, 256):
            n = min(256, nct * 128 - q)
            st = wst.get()
            p.dma(st[:, :, :n], win_v[:, :, q:q + n])
            p.copy(winb[:, :, q:q + n], st[:, :, :n], eng="act")
        xc = Ring(p, [128, 8, 512], F32, "xc", 2)
        hb = Ring(p, [128, 8, 512], BF16, "hb", 2)
        sqr = Ring(p, [128, 512], BF16, "sq", 3)
        hfr = Ring(p, [128, 512], F32, "hf", 3)
        ust = Ring(p, [128, 512], F32, "ust", 4)
        rstd = p.sbuf([128, 512], F32, "rstd"); rtmp = p.sbuf([128, 512], F32, "rtmp")
        chunks = [(1, 0, n_ctx)] + [(0, o, 512) for o in range(0, n_lat, 512)]
        for (w, o, n) in chunks:
            src = (xT if w == 0 else xcT).re("(k p) t -> p k t", p=128)
            x_ = xc.get()
            p.dma(x_[:, :, :n], src[:, :, o:o + n])
            pss = banks[6]
            for i in range(8):
                sq = sqr.get()
                p.act(sq[:, :n], x_[:, i, :n], AF.Square)
                p.mm(pss[:, :n], onesb, sq[:, :n], start=(i == 0), stop=(i == 7))
            p.act(rtmp[:, :n], pss[:, :n], AF.Sqrt, bias=epsr, scale=1.0 / D)
            p.recip(rstd[:, :n], rtmp[:, :n])
            h_ = hb.get()
            for i in range(8):
                hf = hfr.get()
                p.tt(hf[:, :n], x_[:, i, :n], rstd[:, :n], ALU.mult, eng="dve" if i % 2 else "pool")
                p.ts(h_[:, i, :n], hf[:, :n], modT[:, 8 + i, w:w + 1], ALU.mult, modT[:, i, w:w + 1], ALU.add,
                     eng="pool" if i % 2 else "dve")
            tdst = (0 if w == 1 else n_ctx) + o
            for ct in range(nct):
                if w == 1 and ct >= 3 * n_hp:
                    continue
                pu = banks[ct % 4]
                for k in range(8):
                    p.mm(pu[:, :n], winb[:, k, ct * 128:(ct + 1) * 128], h_[:, k, :n], start=(k == 0), stop=(k == 7))
                us = ust.get()
                p.copy(us[:, :n], pu[:, :n], eng="act" if ct % 2 else "dve")
                p.dma(uT[ct * 128:(ct + 1) * 128, tdst:tdst + n], us[:, :n])
    if stop <= 0:
        p.finish(); return nc

    with p.phase():
        cosT = p.sbuf([128, n_lat], F32, "cosT"); sinT = p.sbuf([128, n_lat], F32, "sinT")
        perm = p.sbuf([128, 128], F32, "perm"); qknt = p.sbuf([128, 2], F32, "qknt")
        p.dma(cosT, cosd); p.dma(sinT, sind); p.dma(perm, permd); p.dma(qknt, qkn)
        p.ts(qknt[:, 0:1], qknt[:, 0:1], 0.125, ALU.mult)
        onesk = p.sbuf([128, 64], BF16, "onesk"); p.memset(onesk, 1.0)
        identb = p.sbuf([128, 128], BF16, "identb"); p.copy(identb, ident)
        biasT = p.sbuf([128, nvar, 5, 2, 128], F32, "biasT")
        qb = p.sbuf([128, n_lat], BF16, "qb"); kb = p.sbuf([128, TT], BF16, "kb")
        vtm = p.sbuf([128, TT // 128, 128], BF16, "vtm")
        T = {}

        def tmp(name, shape=(128, 512), dt=F32, n=2):
            if name not in T:
                T[name] = Ring(p, list(shape), dt, "n_" + name, n)
            return T[name].get()
        for ihp in range(n_hp):
            bv = biasd.re("k (v j h q) -> k v j h q", v=nvar, j=5, h=2 * n_hp)
            for v_ in range(nvar):
                for jt in range(5):
                    p.dma(biasT[:, v_, jt, :, :], bv[:, v_, jt, 2 * ihp:2 * ihp + 2, :])
            for (w, o, n) in [(1, 0, n_ctx)] + [(0, o_, 512) for o_ in range(0, n_lat, 512)]:
                tsrc = (0 if w == 1 else n_ctx) + o
                for si in range(2):
                    if si == 0 and w == 1:
                        continue
                    x_ = tmp("x")
                    p.dma(x_[:, :n], uT[(si * n_hp + ihp) * 128:(si * n_hp + ihp + 1) * 128, tsrc:tsrc + n])
                    sq = tmp("sq")
                    p.act(sq[:, :n], x_[:, :n], AF.Square)
                    ps_ = banks[6]
                    p.mm(ps_[:, :n], bones, sq[:, :n])
                    r_ = tmp("r")
                    p.act(r_[:, :n], ps_[:, :n], AF.Sqrt, bias=epsr, scale=1.0 / 64)
                    p.recip(r_[:, :n], r_[:, :n])
                    xn = tmp("xn")
                    p.stt(xn[:, :n], x_[:, :n], qknt[:, si:si + 1], r_[:, :n], ALU.mult, ALU.mult)
                    if w == 1:
                        p.copy(kb[:, 0:n_ctx], xn[:, :n], eng="act")
                        continue
                    pp = banks[7]
                    p.mm(pp[:, :n], perm, xn[:, :n])
                    a_ = tmp("a"); b_ = tmp("b")
                    p.tt(a_[:, :n], xn[:, :n], cosT[:, o:o + n], ALU.mult, eng="pool")
                    p.tt(b_[:, :n], pp[:, :n], sinT[:, o:o + n], ALU.mult)
                    dst = qb[:, o:o + n] if si == 0 else kb[:, n_ctx + o:n_ctx + o + n]
                    p.tt(dst, a_[:, :n], b_[:, :n], ALU.add, eng="pool")
                v_ = tmp("v")
                p.dma(v_[:, :n], uT[(2 * n_hp + ihp) * 128:(2 * n_hp + ihp + 1) * 128, tsrc:tsrc + n])
                vb = tmp("vb", (128, 512), BF16)
                p.copy(vb[:, :n], v_[:, :n], eng="act")
                pt = banks[5]
                ptb = V(pt.ap.bitcast(BF16), pt.buf)
                for tt_ in range(n // 128):
                    p.tr(ptb[:, tt_ * 128:(tt_ + 1) * 128], vb[:, tt_ * 128:(tt_ + 1) * 128], identb)
                p.copy(vtm[:, tsrc // 128:tsrc // 128 + n // 128, :],
                       ptb[:, 0:n].re("p (a b) -> p a b", b=128), eng="dve")
            nct_t = n_ctx // 128
            for i in range(nqb):
                var, j0 = blocks[i]
                tiles = [nct_t + j0 + jt for jt in range(5)] + list(range(nct_t))
                nk = len(tiles)
                PT_ = tmp("PT", (128, 2, 7, 128), BF16)
                for h in range(2):
                    hs = slice(64 * h, 64 * h + 64)
                    pS = [banks[2 * h], banks[2 * h + 1]]
                    for ki, tl in enumerate(tiles):
                        bk = pS[ki // 4]
                        p.mm(bk[:, (ki % 4) * 128:(ki % 4 + 1) * 128], kb[hs, tl * 128:(tl + 1) * 128], qb[hs, i * 128:(i + 1) * 128])
                    sb_ = tmp("sb", (128, 5, 128))
                    p.tt(sb_[:, 0:4, :], pS[0][:, 0:512].re("p (a b) -> p a b", b=128), biasT[:, var, 0:4, h, :], ALU.add)
                    p.tt(sb_[:, 4:5, :], pS[1][:, 0:128].re("p (a b) -> p a b", b=128), biasT[:, var, 4:5, h, :], ALU.add)
                    p.act(PT_[:, h, 0:5, :], sb_, AF.Exp)
                    p.act(PT_[:, h, 5:5 + nct_t, :], pS[1][:, 128:128 + nct_t * 128].re("p (a b) -> p a b", b=128), AF.Exp)
                pN, pD = banks[4], banks[5]
                for h in range(2):
                    hs = slice(64 * h, 64 * h + 64)
                    for ki, tl in enumerate(tiles):
                        p.mm(pN[hs, 0:128], vtm[:, tl, hs], PT_[:, h, ki, :], start=(ki == 0), stop=(ki == nk - 1))
                    for ki, tl in enumerate(tiles):
                        p.mm(pD[hs, 0:128], onesk, PT_[:, h, ki, :], start=(ki == 0), stop=(ki == nk - 1))
                rd = tmp("rd", (128, 128))
                p.recip(rd, pD[:, 0:128])
                o_ = tmp("o", (128, 128))
                p.tt(o_, pN[:, 0:128], rd, ALU.mult)
                p.dma(ymix[ihp * 128:(ihp + 1) * 128, i * 128:(i + 1) * 128], o_)
    if stop <= 1:
        p.finish(); return nc

    with p.phase():
        T = {}

        def tmp(name, shape=(128, 512), dt=F32, n=2):
            if name not in T:
                T[name] = Ring(p, list(shape), dt, "f_" + name, n)
            return T[name].get()
        w1t = p.sbuf([33, 64], F32, "w1t"); w2t = p.sbuf([64, 64], F32, "w2t"); hv = p.sbuf([64, 4], F32, "hv")
        w3t = p.sbuf([64, 4 * n_ht * 128], F32, "w3t"); delt = p.sbuf([128, n_ht], F32, "delt")
        p.dma(w1t, hyw1); p.dma(w2t, hyw2); p.dma(hv, hyv); p.dma(w3t, hyw3); p.dma(delt, hdel)
        ndel = p.sbuf([128, n_ht], F32, "ndel"); p.ts(ndel, delt, -1.0, ALU.mult)
        tapst = p.sbuf([128, n_ht * 3, 3], F32, "tapst")
        p.dma(tapst, taps.re("p (a b) -> p a b", b=3))
        PI = math.pi

        def sin_layer(out, ps_, bcol, fcol, n):
            a = tmp("arg", (64, 512))
            p.ts(a[:, :n], ps_, bcol, ALU.add, fcol, ALU.mult)
            c = tmp("cmp", (64, 512))
            for _ in range(2):
                p.ts(c[:, :n], a[:, :n], PI, ALU.is_gt)
                p.stt(a[:, :n], c[:, :n], -2 * PI, a[:, :n], ALU.mult, ALU.add)
                p.ts(c[:, :n], a[:, :n], -PI, ALU.is_lt)
                p.stt(a[:, :n], c[:, :n], 2 * PI, a[:, :n], ALU.mult, ALU.add)
            p.act(out, a[:, :n], AF.Sin)

        for side in range(2):
            zsrc = zTd if side == 0 else zTrd
            tsrc = tnd if side == 0 else tnrd
            for o_ in range(0, n_lat, 512):
                n = 512
                zt = tmp("zt", (33, 512))
                p.dma(zt, zsrc[:, o_:o_ + n])
                tb_ = tmp("tb")
                p.dma(tb_, tsrc[:, o_:o_ + n].with_ap(tsrc.ap[:, o_:o_ + n].partition_broadcast(128)))
                ph = banks[0]
                p.mm(ph[0:64, :n], w1t, zt)
                h1 = tmp("h1", (64, 512))
                sin_layer(h1, ph[0:64, :n], hv[:, 0:1], hv[:, 1:2], n)
                ph2 = banks[1]
                p.mm(ph2[0:64, :n], w2t, h1)
                h2 = tmp("h2", (64, 512))
                sin_layer(h2, ph2[0:64, :n], hv[:, 2:3], hv[:, 3:4], n)
                for ht in range(n_ht):
                    dec = tmp("dec")
                    p.act(dec, tb_, AF.Exp, scale=ndel[:, ht:ht + 1])
                    for o2 in range(2):
                        col = ((o2 * 2 + side) * n_ht + ht) * 128
                        pf = banks[2 + (o2 + 2 * ht) % 4]
                        p.mm(pf[:, :n], w3t[:, col:col + 128], h2)
                        fo = tmp("fo")
                        p.tt(fo, pf[:, :n], dec, ALU.mult)
                        p.dma(filt[col:col + 128, o_:o_ + n], fo)
                        if side == 1 and o_ == 0:
                            pass
        zt = tmp("zt", (33, 512))
        p.dma(zt[:, 0:2], zTd[:, 0:2])
        ph = banks[0]
        p.mm(ph[0:64, 0:2], w1t, zt[:, 0:2])
        h1 = tmp("h1", (64, 512))
        sin_layer(h1[:, 0:2], ph[0:64, 0:2], hv[:, 0:1], hv[:, 1:2], 2)
        ph2 = banks[1]
        p.mm(ph2[0:64, 0:2], w2t, h1[:, 0:2])
        h2 = tmp("h2", (64, 512))
        sin_layer(h2[:, 0:2], ph2[0:64, 0:2], hv[:, 2:3], hv[:, 3:4], 2)
        for ht in range(n_ht):
            for o2 in range(2):
                col = ((o2 * 2 + 1) * n_ht + ht) * 128
                pf = banks[2]
                p.mm(pf[:, 0:2], w3t[:, col:col + 128], h2[:, 0:2])
                fo = tmp("fo")
                p.copy(fo[:, 0:2], pf[:, 0:2])
                r0 = (o2 * n_ht + ht) * 128
                p.dma(hb0d[r0:r0 + 128, :], fo[:, 0:1])
        for s3 in range(3):
            for ht in range(n_ht):
                ct = 3 * n_hp + s3 * n_ht + ht
                for o_ in range(0, n_lat, 512):
                    uh = tmp("uh", (128, 514))
                    a = 1 if o_ == 0 else 0
                    b = 513 if o_ + 512 >= n_lat else 514
                    if a:
                        p.memset(uh[:, 0:1], 0.0, eng="pool")
                    if b == 513:
                        p.memset(uh[:, 513:514], 0.0, eng="pool")
                    p.dma(uh[:, a:b], uT[ct * 128:(ct + 1) * 128, n_ctx + o_ - 1 + a:n_ctx + o_ - 1 + b])
                    tp = tapst[:, s3 * n_ht + ht, :]
                    oc = tmp("oc")
                    p.ts(oc, uh[:, 1:513], tp[:, 1:2], ALU.mult, eng="pool")
                    p.stt(oc, uh[:, 0:512], tp[:, 0:1], oc, ALU.mult, ALU.add)
                    p.stt(oc, uh[:, 2:514], tp[:, 2:3], oc, ALU.mult, ALU.add)
                    r0 = (s3 * n_ht + ht) * 128
                    p.dma(hcv[r0:r0 + 128, o_:o_ + 512], oc)
    if stop <= 2:
        p.finish(); return nc

    with p.phase():
        NR = n_lat // 128
        dftf = p.sbuf([128, 3, 128], F32, "dftf")
        p.dma(dftf, dftd.re("(a p) k -> p a k", p=128))
        Fr = p.sbuf([128, 128], BF16, "Fr"); Fi = p.sbuf([128, 128], BF16, "Fi"); nFi = p.sbuf([128, 128], BF16, "nFi")
        p.copy(Fr, dftf[:, 0, :]); p.copy(Fi, dftf[:, 1, :]); p.copy(nFi, dftf[:, 2, :])
        FrFi = p.sbuf([128, 256], BF16, "FrFi"); FrnFi = p.sbuf([128, 256], BF16, "FrnFi"); FiFr = p.sbuf([128, 256], BF16, "FiFr")
        p.copy(FrFi[:, 0:128], dftf[:, 0, :]); p.copy(FrFi[:, 128:256], dftf[:, 1, :])
        p.copy(FrnFi[:, 0:128], dftf[:, 0, :]); p.copy(FrnFi[:, 128:256], dftf[:, 2, :])
        p.copy(FiFr[:, 0:128], dftf[:, 1, :]); p.copy(FiFr[:, 128:256], dftf[:, 0, :])
        Tr = p.sbuf([128, 128], F32, "Tr"); Ti = p.sbuf([128, 128], F32, "Ti"); nTi = p.sbuf([128, 128], F32, "nTi")
        p.dma(Tr, Trd); p.dma(Ti, Tid); p.ts(nTi, Ti, -1.0, ALU.mult)
        T = {}

        def tmp(name, shape, dt=F32, n=2):
            if name not in T:
                T[name] = Ring(p, list(shape), dt, "y_" + name, n)
            return T[name].get()

        def bc2(t):
            return t.with_ap(t.ap.unsqueeze(1).to_broadcast([128, 2, 128]))

        def fft_stage1(X, K, Yr, Yi, tr, ti):
            for c2 in range(0, CG, 2):
                bk = banks[(c2 // 2) % 4]
                for c in range(2):
                    p.mm(bk[:, c * 256:(c + 1) * 256], X[0:K, c2 + c, :], FrFi[0:K, :])
                y = bk[:, 0:512].re("p (c r k) -> p c r k", c=2, r=2)
                yr, yi = y[:, :, 0, :], y[:, :, 1, :]
                t1 = tmp("t1", (128, 2, 128)); t2 = tmp("t2", (128, 2, 128))
                t3 = tmp("t3", (128, 2, 128)); t4 = tmp("t4", (128, 2, 128))
                p.tt(t1, yr, bc2(tr), ALU.mult)
                p.tt(t2, yi, bc2(ti), ALU.mult)
                p.tt(t3, yr, bc2(ti), ALU.mult)
                p.tt(t4, yi, bc2(tr), ALU.mult)
                p.tt(Yr[:, c2:c2 + 2, :], t1, t2, ALU.subtract, eng="pool")
                p.tt(Yi[:, c2:c2 + 2, :], t3, t4, ALU.add, eng="pool")

        def fft_stage2(Yr, Yi, c4):
            bR, bI = banks[4 + (c4 // 4) % 2 * 2], banks[5 + (c4 // 4) % 2 * 2]
            yr = Yr[:, c4:c4 + 4, :].re("p c k -> p (c k)"); yi = Yi[:, c4:c4 + 4, :].re("p c k -> p (c k)")
            p.mm(bR[:, 0:512], Fr, yr, start=True, stop=False)
            p.mm(bR[:, 0:512], nFi, yi, start=False, stop=True)
            p.mm(bI[:, 0:512], Fi, yr, start=True, stop=False)
            p.mm(bI[:, 0:512], Fr, yi, start=False, stop=True)
            return bR[:, 0:512].re("p (c k) -> p c k", c=4), bI[:, 0:512].re("p (c k) -> p c k", c=4)

        ngrp = n_ht * 128 // CG
        for g in range(ngrp):
            ht, c0 = (g * CG) // 128, (g * CG) % 128
            Kf = []
            for o2 in range(2):
                K2 = tmp("K2", (128, CG, 128))
                if NR < 64:
                    p.memset(K2, 0.0, eng="pool")
                for side in range(2):
                    r0 = ((o2 * 2 + side) * n_ht + ht) * 128 + c0
                    src = filt[r0:r0 + CG, :].re("c (a b) -> a c b", b=128)
                    rr = 0 if side == 0 else 128 - NR
                    p.dma(K2[rr:rr + NR, :, :], src)
                hb0 = tmp("hb0", (1, 2, CG))
                r0 = (o2 * n_ht + ht) * 128 + c0
                p.dma(hb0[:, 0, :], hb0d[r0:r0 + CG, :].re("c o -> o c"))
                bcol = (o2 * n_ht + ht) * 128 + c0
                p.dma(hb0[:, 1, :], hybias[:, bcol:bcol + CG])
                p.tt(K2[0:1, :, 0], K2[0:1, :, 0], hb0[:, 0, :], ALU.add)
                p.tt(K2[0:1, :, 0], K2[0:1, :, 0], hb0[:, 1, :], ALU.add)
                K2b = tmp("K2b", (128, CG, 128), BF16)
                p.copy(K2b, K2, eng="act")
                Yr = tmp("Yr", (128, CG, 128), BF16); Yi = tmp("Yi", (128, CG, 128), BF16)
                fft_stage1(K2b, 128, Yr, Yi, Tr, Ti)
                kfr = tmp("Kfr%d" % o2, (128, CG, 128), BF16, 1); kfi = tmp("Kfi%d" % o2, (128, CG, 128), BF16, 1)
                for c4 in range(0, CG, 4):
                    zr, zi = fft_stage2(Yr, Yi, c4)
                    p.copy(kfr[:, c4:c4 + 4, :], zr, eng="act")
                    p.copy(kfi[:, c4:c4 + 4, :], zi, eng="act")
                Kf.append((kfr, kfi))
            xs = []
            for s3 in range(3):
                x_ = tmp("x%d" % s3, (64, CG, 128), F32, 1)
                r0 = (s3 * n_ht + ht) * 128 + c0
                p.dma(x_[0:NR, :, :], hcv[r0:r0 + CG, :].re("c (a b) -> a c b", b=128))
                xs.append(x_)
            cur = tmp("xb", (64, CG, 128), BF16)
            p.copy(cur[0:NR], xs[0][0:NR], eng="act")
            for o2 in range(2):
                Yr = tmp("Yr", (128, CG, 128), BF16); Yi = tmp("Yi", (128, CG, 128), BF16)
                fft_stage1(cur, NR, Yr, Yi, Tr, Ti)
                Zr_ = tmp("Zr", (128, CG, 128), BF16); Zi_ = tmp("Zi", (128, CG, 128), BF16)
                kfr, kfi = Kf[o2]
                for c4 in range(0, CG, 4):
                    zr, zi = fft_stage2(Yr, Yi, c4)
                    s4 = (128, 4, 128)
                    t1 = tmp("u1", s4); t2 = tmp("u2", s4); t3 = tmp("u3", s4); t4 = tmp("u4", s4)
                    p.tt(t1, zr, kfr[:, c4:c4 + 4, :], ALU.mult)
                    p.tt(t2, zi, kfi[:, c4:c4 + 4, :], ALU.mult)
                    p.tt(t3, zr, kfi[:, c4:c4 + 4, :], ALU.mult)
                    p.tt(t4, zi, kfr[:, c4:c4 + 4, :], ALU.mult)
                    p.tt(Zr_[:, c4:c4 + 4, :], t1, t2, ALU.subtract, eng="pool")
                    p.tt(Zi_[:, c4:c4 + 4, :], t3, t4, ALU.add, eng="pool")
                Dr = tmp("Dr", (128, CG, 128), BF16); Di = tmp("Di", (128, CG, 128), BF16)
                for c2 in range(0, CG, 2):
                    bk = banks[(c2 // 2) % 4]
                    for c in range(2):
                        p.mm(bk[:, c * 256:(c + 1) * 256], Zr_[:, c2 + c, :], FrnFi, start=True, stop=False)
                        p.mm(bk[:, c * 256:(c + 1) * 256], Zi_[:, c2 + c, :], FiFr, start=False, stop=True)
                    y = bk[:, 0:512].re("p (c r k) -> p c r k", c=2, r=2)
                    yr, yi = y[:, :, 0, :], y[:, :, 1, :]
                    t1 = tmp("t1", (128, 2, 128)); t2 = tmp("t2", (128, 2, 128))
                    t3 = tmp("t3", (128, 2, 128)); t4 = tmp("t4", (128, 2, 128))
                    p.tt(t1, yr, bc2(Tr), ALU.mult)
                    p.tt(t2, yi, bc2(nTi), ALU.mult)
                    p.tt(t3, yr, bc2(nTi), ALU.mult)
                    p.tt(t4, yi, bc2(Tr), ALU.mult)
                    p.tt(Dr[:, c2:c2 + 2, :], t1, t2, ALU.subtract, eng="pool")
                    p.tt(Di[:, c2:c2 + 2, :], t3, t4, ALU.add, eng="pool")
                gate = xs[1 + o2]
                if o2 == 0:
                    nxt = tmp("xb", (64, CG, 128), BF16)
                else:
                    nxt = tmp("yo", (64, CG, 128), F32)
                for c4 in range(0, CG, 4):
                    bk = banks[4 + (c4 // 4) % 4]
                    p.mm(bk[0:NR, 0:512], Fr[:, 0:NR], Dr[:, c4:c4 + 4, :].re("p c k -> p (c k)"), start=True, stop=False)
                    p.mm(bk[0:NR, 0:512], Fi[:, 0:NR], Di[:, c4:c4 + 4, :].re("p c k -> p (c k)"), start=False, stop=True)
                    p.stt(nxt[0:NR, c4:c4 + 4, :], bk[0:NR, 0:512].re("p (c k) -> p c k", c=4), 1.0 / NFFT,
                          gate[0:NR, c4:c4 + 4, :], ALU.mult, ALU.mult)
                cur = nxt
            r0 = (n_hp + ht) * 128 + c0
            p.dma(ymix[r0:r0 + CG, :].re("c (a b) -> a c b", b=128), cur[0:NR, :, :])
    p.finish()
    return nc


C_OFF = dict(q=0, k=512, v=1024, hv=1536, hx1=2048, hx2=2560)


def mix1_cols(pairs, tiles):
    cols = []
    for s in ("q", "k", "v"):
        for g in pairs:
            cols.append(np.arange(C_OFF[s] + g * 128, C_OFF[s] + g * 128 + 128))
    for s in ("hv", "hx1", "hx2"):
        for g in tiles:
            cols.append(np.arange(C_OFF[s] + g * 128, C_OFF[s] + g * 128 + 128))
    return np.concatenate(cols)


def mix1_host_inputs(b, pairs, tiles, inp, xT_b, xcT_b, n_lat=8192):
    m = {}
    cols = mix1_cols(pairs, tiles)
    n_hp, n_ht = len(pairs), len(tiles)
    m["xT"] = np.ascontiguousarray(xT_b); m["xcT"] = np.ascontiguousarray(xcT_b)
    cv = np.stack([inp["c"][b], inp["c_ctx"]], axis=-1)
    m["cvec"] = np.ascontiguousarray(cv.reshape(8, 128, 2).transpose(1, 0, 2))
    m["modw"] = np.ascontiguousarray(inp["mod_w"][1][:, 0:2048])
    m["modb"] = np.ascontiguousarray(inp["mod_b"][1][0:2048].reshape(16, 128).T)
    m["win"] = np.ascontiguousarray(inp["cd_w_in"][0][:, cols])
    qn = np.tile(inp["na_q_norm"][0], 2)
    kn = np.tile(inp["na_k_norm"][0], 2)
    m["qkn"] = np.ascontiguousarray(np.stack([qn, kn], -1).astype(np.float32))
    heads = np.concatenate([[2 * g, 2 * g + 1] for g in pairs])
    bt, _ = na_bias_variants(np.asarray(inp["na_rpb"][0])[heads], n_lat // GRID_W)
    m["biasT"] = np.ascontiguousarray(bt.transpose(3, 0, 1, 2, 4).reshape(128, -1))
    cosT, sinT, perm = rope_tables(n_lat)
    m["cosT"], m["sinT"], m["perm"] = cosT, sinT, perm
    m["ident"] = np.eye(128, dtype=np.float32)
    bo = np.zeros((128, 128), np.float32); bo[:64, :64] = 1; bo[64:, 64:] = 1
    m["bones"] = bo
    hcols = np.concatenate([s * 512 + np.concatenate([np.arange(g * 128, g * 128 + 128) for g in tiles]) for s in range(3)])
    sh = inp["hy_short"][0][:, hcols]
    m["taps"] = np.ascontiguousarray(sh.reshape(3, 3 * n_ht, 128).transpose(2, 1, 0).reshape(128, -1))
    m["hyw1"] = inp["hy_w1"][0]; m["hyw2"] = inp["hy_w2"][0]
    m["hyv"] = np.ascontiguousarray(np.stack([inp["hy_b1"][0], inp["hy_freq1"][0], inp["hy_b2"][0], inp["hy_freq2"][0]], -1))
    w3 = np.asarray(inp["hy_w3"][0]).reshape(64, 2, 2, 512)
    ch = np.concatenate([np.arange(g * 128, g * 128 + 128) for g in tiles])
    m["hyw3"] = np.ascontiguousarray(w3[:, :, :, ch].reshape(64, -1))
    m["hybias"] = np.ascontiguousarray(np.asarray(inp["hy_bias"][0])[:, ch].reshape(1, -1))
    hc = hyena_consts(n_lat)
    m["hdel"] = np.ascontiguousarray(hc["deltas"][ch].reshape(n_ht, 128).T)
    m["zT"], m["zTr"], m["tn"], m["tnr"] = hc["zT"], hc["zTr"], hc["tn"], hc["tnr"]
    m["dft"] = np.ascontiguousarray(hc["dft"].reshape(3 * 128, 128)); m["Tr"], m["Ti"] = hc["Tr"], hc["Ti"]
    return m


_PROGS = {}


def _prog(key, fn):
    if key not in _PROGS:
        _PROGS[key] = fn()
    return _PROGS[key]


def kernel(**inputs):
    inp = {k: np.asarray(v) for k, v in inputs.items()}
    B, S, Dm = inp["x"].shape
    NCTX = inp["ctx"].shape[1]
    cores = list(range(8))
    xT = [np.ascontiguousarray(inp["x"][b].T) for b in range(B)]
    xcT = [np.ascontiguousarray(inp["ctx"][b].T) for b in range(B)]
    perm = np.concatenate([np.arange(0, 2 * D, 2), np.arange(1, 2 * D, 2)])

    nc0 = _prog("mix0", lambda: build_mix0(2, 2, n_lat=S, n_ctx=NCTX))
    maps = [mix0_host_inputs(c // 2, [2 * (c % 2), 2 * (c % 2) + 1], [2 * (c % 2), 2 * (c % 2) + 1], inp, xT[c // 2], xcT[c // 2])
            for c in cores]
    res = run_bass_kernel_spmd(nc0, maps, core_ids=cores).results
    ycat = []
    for b in range(B):
        y = np.empty((D, NCTX + S), np.float32)
        for hh in range(2):
            r = res[2 * b + hh]["ymix"]
            y[hh * 256:(hh + 1) * 256] = r[0:256]
            y[512 + hh * 256:512 + (hh + 1) * 256] = r[256:512]
        ycat.append(y)
    del res, maps

    inp["_w1p"] = {0: np.ascontiguousarray(inp["moe_w1"][0][:, :, perm])}
    ncf0 = _prog("ffn0", lambda: build_ffn(S // 2, NCTX))
    maps = [ffn_host_inputs(0, c, inp, xT[c // 2], ycat[c // 2][:, NCTX:], xcT[c // 2], ycat[c // 2][:, :NCTX], n_lat=S // 2)
            for c in cores]
    res = run_bass_kernel_spmd(ncf0, maps, core_ids=cores).results
    x1T = [np.concatenate([res[2 * b]["xoT"], res[2 * b + 1]["xoT"]], axis=1) for b in range(B)]
    xc1T = [res[2 * b]["xocT"] for b in range(B)]
    del res, maps, ycat
    inp["_w1p"] = {}

    nc1 = _prog("mix1", lambda: build_mix1(2, 2, n_lat=S, n_ctx=NCTX))
    maps = [mix1_host_inputs(c // 2, [2 * (c % 2), 2 * (c % 2) + 1], [2 * (c % 2), 2 * (c % 2) + 1], inp, x1T[c // 2], xc1T[c // 2], n_lat=S)
            for c in cores]
    res = run_bass_kernel_spmd(nc1, maps, core_ids=cores).results
    ycat = []
    for b in range(B):
        y = np.empty((D, S), np.float32)
        for hh in range(2):
            r = res[2 * b + hh]["ymix"]
            y[hh * 256:(hh + 1) * 256] = r[0:256]
            y[512 + hh * 256:512 + (hh + 1) * 256] = r[256:512]
        ycat.append(y)
    del res, maps

    inp["_w1p"] = {1: np.ascontiguousarray(inp["moe_w1"][1][:, :, perm])}
    ncf1 = _prog("ffn1", lambda: build_ffn(S // 2, 0))
    maps = [ffn_host_inputs(1, c, inp, x1T[c // 2], ycat[c // 2], None, None, n_lat=S // 2) for c in cores]
    res = run_bass_kernel_spmd(ncf1, maps, core_ids=cores).results
    out = np.empty((B, S, D), np.float32)
    for b in range(B):
        out[b, :S // 2] = res[2 * b]["xoT"].T
        out[b, S // 2:] = res[2 * b + 1]["xoT"].T
    return out
```
